# Optimizing a Trainium2 kernel written in Bass

```python
import jax, jax.numpy as jnp
from jax import lax
import numpy as np

D_MODEL = 1024
BATCH = 16
SEQ = 4096
DEPTH = 2

N_META = 16
A_HEADS = 4
A_QK_DIM = 128
A_V_DIM = 256
A_CHUNK = 64
GATE_PAD = -1e30
B_HEADS = 8
B_HEAD_DIM = 128
B_BLOCK = 128
D_FF_DENSE = 2816
N_EXPERTS = 8
TOP_K = 2
D_FF_EXPERT = 3584
MOE_BLOCK = 256
N_A_LAYERS = DEPTH // 2
N_B_LAYERS = DEPTH - N_A_LAYERS
N_DENSE_LAYERS = (DEPTH + 1) // 2
N_MOE_LAYERS = DEPTH // 2
DEEPNORM_ALPHA = (2 * DEPTH) ** 0.25
DEEPNORM_BETA = (8 * DEPTH) ** -0.25
LN_EPS = 1e-5

kernel_name = "yoco_mlstm_stickbreak_moe_deepnorm"


def _layernorm(x, g, b):
    xf = x.astype(jnp.float32)
    mu = xf.mean(-1, keepdims=True)
    var = jnp.mean(jnp.square(xf - mu), -1, keepdims=True)
    return ((xf - mu) * lax.rsqrt(var + LN_EPS) * g.astype(jnp.float32) + b.astype(jnp.float32)).astype(x.dtype)


def _mlstm_chunk(carry, xs):
    c_mat, n_vec, m = carry
    q, k, v, li, lf = xs
    b = jnp.cumsum(lf, axis=-1)
    causal = jnp.tril(jnp.ones((A_CHUNK, A_CHUNK), dtype=bool))
    d_log = jnp.where(causal, b[..., :, None] - b[..., None, :] + li[..., None, :], -jnp.inf)
    inter = b + m[..., None]
    m_t = jnp.maximum(inter, d_log.max(-1))
    w_intra = jnp.exp(d_log - m_t[..., None])
    w_inter = jnp.exp(inter - m_t)
    s = jnp.einsum('bhtd,bhsd->bhts', q, k) * w_intra
    num = w_inter[..., None] * jnp.einsum('bhtd,bhde->bhte', q, c_mat) + jnp.einsum('bhts,bhse->bhte', s, v)
    den = w_inter * jnp.einsum('bhtd,bhd->bht', q, n_vec) + s.sum(-1)
    h = num / jnp.maximum(jnp.abs(den), jnp.exp(-m_t))[..., None]
    b_last = b[..., -1]
    g = b_last[..., None] - b + li
    m_new = jnp.maximum(b_last + m, g.max(-1))
    decay = jnp.exp(b_last + m - m_new)
    wk = jnp.exp(g - m_new[..., None])[..., None] * k
    c_new = decay[..., None, None] * c_mat + jnp.einsum('bhsd,bhse->bhde', wk, v)
    n_new = decay[..., None] * n_vec + wk.sum(2)
    return (c_new, n_new, m_new), h


def _mlstm_mixer(h, w_in, b_gate, norm_g, w_out):
    bsz, length, _ = h.shape
    hk, hv = A_HEADS * A_QK_DIM, A_HEADS * A_V_DIM
    proj = h @ w_in
    q = proj[..., :hk]
    k = proj[..., hk:2 * hk]
    v = proj[..., 2 * hk:2 * hk + hv]
    o = proj[..., 2 * hk + hv:2 * hk + 2 * hv]
    gates = (proj[..., 2 * hk + 2 * hv:] + b_gate).astype(jnp.float32)
    li = gates[..., :A_HEADS].transpose(0, 2, 1)
    lf = jax.nn.log_sigmoid(gates[..., A_HEADS:]).transpose(0, 2, 1)

    def heads(t, dh):
        return t.reshape(bsz, length, A_HEADS, dh).transpose(0, 2, 1, 3).astype(jnp.float32)

    qh = heads(q, A_QK_DIM)
    kh = heads(k, A_QK_DIM) * (A_QK_DIM ** -0.5)
    vh = heads(v, A_V_DIM)
    pad = A_CHUNK - N_META

    def padseq(t, val):
        return jnp.pad(t, [(0, 0), (0, 0), (pad, 0)] + [(0, 0)] * (t.ndim - 3), constant_values=val)

    n_chunks = (length + pad) // A_CHUNK

    def chunks(t):
        return jnp.moveaxis(t.reshape((bsz, A_HEADS, n_chunks, A_CHUNK) + t.shape[3:]), 2, 0)

    xs = (chunks(padseq(qh, 0.0)), chunks(padseq(kh, 0.0)), chunks(padseq(vh, 0.0)),
          chunks(padseq(li, GATE_PAD)), chunks(padseq(lf, 0.0)))
    init = (jnp.zeros((bsz, A_HEADS, A_QK_DIM, A_V_DIM), jnp.float32),
            jnp.zeros((bsz, A_HEADS, A_QK_DIM), jnp.float32),
            jnp.zeros((bsz, A_HEADS), jnp.float32))
    _, hs = lax.scan(_mlstm_chunk, init, xs)
    hs = jnp.moveaxis(hs, 0, 2).reshape(bsz, A_HEADS, n_chunks * A_CHUNK, A_V_DIM)[:, :, pad:]
    hs = hs.transpose(0, 2, 1, 3)
    mu = hs.mean(-1, keepdims=True)
    var = jnp.mean(jnp.square(hs - mu), -1, keepdims=True)
    hn = (hs - mu) * lax.rsqrt(var + LN_EPS) * norm_g.astype(jnp.float32).reshape(A_HEADS, A_V_DIM)
    out = jax.nn.sigmoid(o.astype(jnp.float32)) * hn.reshape(bsz, length, hv)
    return out.astype(h.dtype) @ w_out


def _shared_kv(h, w_kv):
    bsz, length, _ = h.shape
    kv = (h @ w_kv).reshape(bsz, length, 2, B_HEADS, B_HEAD_DIM).transpose(2, 0, 3, 1, 4).astype(jnp.float32)
    kv = jnp.pad(kv, [(0, 0), (0, 0), (0, 0), (B_BLOCK - N_META, 0), (0, 0)])
    return kv[0], kv[1]


def _stick_breaking_mixer(h, w_q, k_sh, v_sh, w_o):
    bsz, length, _ = h.shape
    pad = B_BLOCK - N_META
    q = (h @ w_q).reshape(bsz, length, B_HEADS, B_HEAD_DIM).transpose(0, 2, 1, 3).astype(jnp.float32)
    q = jnp.pad(q, [(0, 0), (0, 0), (pad, 0), (0, 0)]) * (B_HEAD_DIM ** -0.5)
    total = length + pad
    n_blk = total // B_BLOCK
    qb = jnp.moveaxis(q.reshape(bsz, B_HEADS, n_blk, B_BLOCK, B_HEAD_DIM), 2, 0)
    kpos = jnp.arange(total)

    def block(args):
        q_blk, blk = args
        qpos = blk * B_BLOCK + jnp.arange(B_BLOCK)
        z = jnp.einsum('bhqd,bhkd->bhqk', q_blk, k_sh)
        valid = (kpos[None, :] < qpos[:, None]) & (kpos[None, :] >= pad)
        sp = jnp.where(valid, jax.nn.softplus(z), 0.0)
        suffix = lax.cumsum(sp, axis=3, reverse=True) - sp
        a = jnp.where(valid, jnp.exp(jax.nn.log_sigmoid(z) - suffix), 0.0)
        return jnp.einsum('bhqk,bhkd->bhqd', a, v_sh)

    ob = lax.map(block, (qb, jnp.arange(n_blk)))
    o = jnp.moveaxis(ob, 0, 2).reshape(bsz, B_HEADS, total, B_HEAD_DIM)[:, :, pad:]
    o = o.transpose(0, 2, 1, 3).reshape(bsz, length, B_HEADS * B_HEAD_DIM).astype(h.dtype)
    return o @ w_o


def _swiglu(x, w_gate, w_up, w_down):
    return (jax.nn.silu(x @ w_gate) * (x @ w_up)) @ w_down


def _moe_swiglu(x2, w_router, w_gate, w_up, w_down):
    n = x2.shape[0]
    logits = x2.astype(jnp.float32) @ w_router.astype(jnp.float32)
    top_val, top_idx = lax.top_k(logits, TOP_K)
    gates = jax.nn.softmax(top_val, axis=-1)
    flat_e = top_idx.reshape(-1)
    flat_tok = jnp.repeat(jnp.arange(n, dtype=jnp.int32), TOP_K)
    flat_g = gates.reshape(-1)
    order = jnp.argsort(flat_e)
    se, stok, sg = flat_e[order], flat_tok[order], flat_g[order]
    counts = jnp.bincount(flat_e, length=N_EXPERTS)
    start = jnp.cumsum(counts) - counts
    pcounts = (counts + MOE_BLOCK - 1) // MOE_BLOCK * MOE_BLOCK
    pend = jnp.cumsum(pcounts)
    pstart = pend - pcounts
    dest = pstart[se] + (jnp.arange(n * TOP_K) - start[se])
    n_blk = -(-(n * TOP_K + N_EXPERTS * (MOE_BLOCK - 1)) // MOE_BLOCK)
    rows = n_blk * MOE_BLOCK
    tok_buf = jnp.full((rows,), n, dtype=jnp.int32).at[dest].set(stok)
    x_pad = jnp.concatenate([x2, jnp.zeros((1, x2.shape[1]), x2.dtype)], axis=0)
    xb = x_pad[tok_buf].reshape(n_blk, MOE_BLOCK, x2.shape[1])
    blk_e = jnp.minimum(jnp.searchsorted(pend, jnp.arange(n_blk) * MOE_BLOCK, side='right'), N_EXPERTS - 1)

    def expert_block(args):
        x_blk, e = args
        return _swiglu(x_blk, w_gate[e], w_up[e], w_down[e])

    yb = lax.map(expert_block, (xb, blk_e)).reshape(rows, x2.shape[1])
    y = yb[dest] * sg[:, None].astype(yb.dtype)
    return jnp.zeros_like(x2).at[stok].add(y)


def setup_inputs(seed: int = 0) -> dict:
    key = jax.random.key(seed)
    ks = jax.random.split(key, 20)
    f32 = jnp.float32
    hk, hv = A_HEADS * A_QK_DIM, A_HEADS * A_V_DIM
    a_in = 2 * hk + 2 * hv + 2 * A_HEADS
    bw = B_HEADS * B_HEAD_DIM

    def nrm(k, shape, fan_in, scale=1.0):
        return jax.random.normal(k, shape, f32) * (fan_in ** -0.5) * scale

    f_bias = jnp.linspace(3.0, 6.0, A_HEADS, dtype=f32)
    b_gate = jnp.concatenate([jnp.zeros((N_A_LAYERS, A_HEADS), f32),
                              jnp.broadcast_to(f_bias, (N_A_LAYERS, A_HEADS))], axis=-1)
    b_gate = b_gate + 0.1 * jax.random.normal(ks[3], (N_A_LAYERS, 2 * A_HEADS), f32)
    return {
        "x": jax.random.normal(ks[0], (BATCH, SEQ, D_MODEL), f32),
        "meta": jax.random.normal(ks[1], (N_META, D_MODEL), f32),
        "w_in_a": nrm(ks[2], (N_A_LAYERS, D_MODEL, a_in), D_MODEL),
        "b_gate_a": b_gate,
        "norm_a": 1.0 + 0.02 * jax.random.normal(ks[4], (N_A_LAYERS, hv), f32),
        "w_out_a": nrm(ks[5], (N_A_LAYERS, hv, D_MODEL), hv, DEEPNORM_BETA),
        "w_kv": nrm(ks[6], (D_MODEL, 2 * bw), D_MODEL),
        "w_q_b": nrm(ks[7], (N_B_LAYERS, D_MODEL, bw), D_MODEL),
        "w_o_b": nrm(ks[8], (N_B_LAYERS, bw, D_MODEL), bw, DEEPNORM_BETA),
        "w_gate_d": nrm(ks[9], (N_DENSE_LAYERS, D_MODEL, D_FF_DENSE), D_MODEL),
        "w_up_d": nrm(ks[10], (N_DENSE_LAYERS, D_MODEL, D_FF_DENSE), D_MODEL),
        "w_down_d": nrm(ks[11], (N_DENSE_LAYERS, D_FF_DENSE, D_MODEL), D_FF_DENSE, DEEPNORM_BETA),
        "w_router": nrm(ks[12], (N_MOE_LAYERS, D_MODEL, N_EXPERTS), D_MODEL),
        "w_gate_e": nrm(ks[13], (N_MOE_LAYERS, N_EXPERTS, D_MODEL, D_FF_EXPERT), D_MODEL),
        "w_up_e": nrm(ks[14], (N_MOE_LAYERS, N_EXPERTS, D_MODEL, D_FF_EXPERT), D_MODEL),
        "w_down_e": nrm(ks[15], (N_MOE_LAYERS, N_EXPERTS, D_FF_EXPERT, D_MODEL), D_FF_EXPERT, DEEPNORM_BETA),
        "ln_g": 1.0 + 0.02 * jax.random.normal(ks[16], (DEPTH, 2, D_MODEL), f32),
        "ln_b": 0.02 * jax.random.normal(ks[17], (DEPTH, 2, D_MODEL), f32),
    }


def reference(x, meta, w_in_a, b_gate_a, norm_a, w_out_a, w_kv, w_q_b, w_o_b,
              w_gate_d, w_up_d, w_down_d, w_router, w_gate_e, w_up_e, w_down_e, ln_g, ln_b):
    bsz = x.shape[0]
    h = jnp.concatenate([jnp.broadcast_to(meta[None].astype(x.dtype), (bsz, N_META, D_MODEL)), x], axis=1)
    k_sh = None
    v_sh = None
    for layer in range(DEPTH):
        if layer < N_A_LAYERS:
            mix = _mlstm_mixer(h, w_in_a[layer], b_gate_a[layer], norm_a[layer], w_out_a[layer])
        else:
            j = layer - N_A_LAYERS
            mix = _stick_breaking_mixer(h, w_q_b[j], k_sh, v_sh, w_o_b[j])
        h = _layernorm(DEEPNORM_ALPHA * h + mix, ln_g[layer, 0], ln_b[layer, 0])
        if layer % 2 == 0:
            i = layer // 2
            ffn = _swiglu(h, w_gate_d[i], w_up_d[i], w_down_d[i])
        else:
            i = layer // 2
            ffn = _moe_swiglu(h.reshape(-1, D_MODEL), w_router[i], w_gate_e[i], w_up_e[i], w_down_e[i]).reshape(h.shape)
        h = _layernorm(DEEPNORM_ALPHA * h + ffn, ln_g[layer, 1], ln_b[layer, 1])
        if layer == N_A_LAYERS - 1:
            k_sh, v_sh = _shared_kv(h, w_kv)
    return h[:, N_META:]
```

```python
import numpy as np
from contextlib import ExitStack
import concourse.bass as bass
import concourse.mybir as mybir
from concourse.bass_utils import run_bass_kernel_spmd

F32 = mybir.dt.float32
BF16 = mybir.dt.bfloat16
I32 = mybir.dt.int32
U32 = mybir.dt.uint32
AF = mybir.ActivationFunctionType
ALU = mybir.AluOpType
AX = mybir.AxisListType

NDSEM = 8
NCORES = 8
D = 1024
SEQ = 4096
NMETA = 16
TPS = 33
RS = TPS * 128
R = 2 * RS
NT = 2 * TPS
PAD = 112
ALPHA = 4.0 ** 0.25
EPS = 1e-5
DFF = 2816
NE = 8
DFE = 3584
CAP = 2304


class Prog:
    ENGS = ("pe", "act", "dve", "pool", "sp")

    def __init__(self, nc):
        self.nc = nc
        self.ges = ExitStack()
        self.es = self.ges
        self.stream = {e: [] for e in self.ENGS}
        self.cnt = {e: 0 for e in self.ENGS}
        self.waited = {e: {} for e in self.ENGS}
        self.res_w = {}
        self.res_r = {}
        self.dq_n = {q: 0 for q in ("sp", "act", "pool")}
        self.sems = {}
        for e in self.ENGS:
            self.sems[("e", e)] = self.ges.enter_context(nc.semaphore("s_e_" + e))
        for q in ("sp", "act", "pool"):
            for i in range(NDSEM):
                self.sems[("d", q, i)] = self.ges.enter_context(nc.semaphore("s_d_%s_%d" % (q, i)))

    def phase_begin(self):
        self.es = ExitStack()

    def barrier(self):
        for e in self.ENGS:
            waits = {}
            for e2 in self.ENGS:
                if e2 != e and self.cnt[e2] > 0:
                    self._need(e, (("e", e2), self.cnt[e2], e2), waits)
            for q in ("sp", "act", "pool"):
                n = self.dq_n[q]
                for slot in range(NDSEM):
                    k = (n - slot + NDSEM - 1) // NDSEM
                    if k > 0:
                        self._need(e, (("d", q, slot), 16 * k, "dma"), waits)
            self.stream[e].append((None, waits, None, 0))

    def phase_end(self, final=False):
        self.barrier()
        self.emit_block()
        if self.es is not self.ges:
            self.es.close()
        self.es = self.ges
        if final:
            self.ges.close()

    def sb(self, name, shape, dtype):
        self.nalloc = getattr(self, "nalloc", 0) + 1
        return self.es.enter_context(self.nc.sbuf_tensor("%s_%d" % (name, self.nalloc), list(shape), dtype))

    def ps(self, name, shape, dtype):
        return self.es.enter_context(self.nc.psum_tensor(name, list(shape), dtype))

    def dram(self, name, shape, dtype, kind="Internal"):
        return self.nc.dram_tensor(name, list(shape), dtype, kind=kind).ap()

    def _need(self, eng, tok, waits):
        if tok is None:
            return
        semkey, val, src = tok
        if src == "pe" and eng == "pe":
            return
        if self.waited[eng].get(semkey, 0) >= val:
            return
        self.waited[eng][semkey] = val
        waits[semkey] = max(waits.get(semkey, 0), val)

    def _deps(self, eng, reads, writes):
        waits = {}
        for r in reads:
            self._need(eng, self.res_w.get(r), waits)
        for w in writes:
            self._need(eng, self.res_w.get(w), waits)
            for t in self.res_r.get(w, ()):
                self._need(eng, t, waits)
        return waits

    def _record(self, tok, reads, writes):
        for r in reads:
            self.res_r.setdefault(r, []).append(tok)
        for w in writes:
            self.res_w[w] = tok
            self.res_r[w] = []

    def op(self, eng, fn, reads=(), writes=(), signal=True):
        waits = self._deps(eng, reads, writes)
        if signal:
            self.cnt[eng] += 1
            tok = (("e", eng), self.cnt[eng], eng)
        else:
            tok = (("e", eng), self.cnt[eng] + 1, eng)
        self._record(tok, reads, writes)
        self.stream[eng].append((fn, waits, ("e", eng) if signal else None, 1))

    def dma(self, q, fn, reads=(), writes=()):
        waits = self._deps(q, reads, writes)
        i = self.dq_n[q]
        self.dq_n[q] += 1
        slot = i % NDSEM
        semkey = ("d", q, slot)
        prev = 16 * (i // NDSEM)
        if prev > 0 and self.waited[q].get(semkey, 0) < prev:
            self.waited[q][semkey] = prev
            waits[semkey] = max(waits.get(semkey, 0), prev)
        tok = (semkey, prev + 16, "dma")
        self._record(tok, reads, writes)
        self.stream[q].append((fn, waits, semkey, 16))

    def wait_all(self, eng, keys):
        waits = {}
        for k in keys:
            self._need(eng, self.res_w.get(k), waits)
        self.stream[eng].append((None, waits, None, 0))

    def mm(self, out, lhsT, rhs, start=True, stop=True, reads=(), writes=(), signal=True):
        self.op("pe", lambda e: e.matmul(out, lhsT, rhs, start=start, stop=stop),
                reads, writes, signal)

    def tr(self, out, in_, ident, reads=(), writes=(), signal=True):
        self.op("pe", lambda e: e.transpose(out, in_, ident), reads, writes, signal)

    def act(self, out, in_, func, bias=None, scale=None, reads=(), writes=()):
        kw = {}
        if bias is not None:
            kw["bias"] = bias
        if scale is not None:
            kw["scale"] = scale
        self.op("act", lambda e: e.activation(out=out, in_=in_, func=func, **kw), reads, writes)

    def tt(self, eng, out, in0, in1, op, reads=(), writes=()):
        self.op(eng, lambda e: e.tensor_tensor(out=out, in0=in0, in1=in1, op=op), reads, writes)

    def ts(self, eng, out, in0, s1, s2, op0, op1=None, reads=(), writes=()):
        if op1 is None:
            self.op(eng, lambda e: e.tensor_scalar(out=out, in0=in0, scalar1=s1, scalar2=None, op0=op0),
                    reads, writes)
        else:
            self.op(eng, lambda e: e.tensor_scalar(out=out, in0=in0, scalar1=s1, scalar2=s2, op0=op0, op1=op1),
                    reads, writes)

    def stt(self, out, in0, scalar, in1, op0, op1, reads=(), writes=()):
        self.op("dve", lambda e: e.scalar_tensor_tensor(out=out, in0=in0, scalar=scalar, in1=in1,
                                                        op0=op0, op1=op1), reads, writes)

    def cp(self, eng, out, in_, reads=(), writes=()):
        if eng == "act":
            self.op("act", lambda e: e.copy(out=out, in_=in_), reads, writes)
        else:
            self.op(eng, lambda e: e.tensor_copy(out=out, in_=in_), reads, writes)

    def ld(self, q, out, in_, reads=(), writes=()):
        self.dma(q, lambda e: e.dma_start(out=out, in_=in_), reads, writes)

    def emit_block(self):
        nc = self.nc
        with nc.Block() as block:
            handles = {"pe": block.tensor, "act": block.scalar, "dve": block.vector,
                       "pool": block.gpsimd, "sp": block.sync}

            def mk(e):
                def body(engh):
                    for fn, waits, sk, inc in self.stream[e]:
                        for k, v in waits.items():
                            engh.wait_ge(self.sems[k], v)
                        if fn is None:
                            continue
                        ins = fn(engh)
                        if sk is not None:
                            ins.then_inc(self.sems[sk], inc)
                return body

            for e in self.ENGS:
                if self.stream[e]:
                    handles[e](mk(e))
        self.stream = {e: [] for e in self.ENGS}


def load_w_bf(p, name, w_ap, K, N, key):
    t = p.sb(name, [128, K // 128, N], BF16)
    src = w_ap.rearrange("(k p) n -> p k n", p=128)
    c0 = 0
    while c0 < N:
        c1 = min(N, c0 + 2048)
        p.ld("pool", t[:, :, c0:c1], src[:, :, c0:c1], writes=[key])
        c0 = c1
    return t


def layernorm_tile(p, y, out, gbc, bbc, sm, key_y, key_out, extra_reads=()):
    st = sm[:, 0:12].rearrange("p (a b) -> p a b", a=2)
    for hf in range(2):
        p.op("dve", lambda e, hf=hf: e.bn_stats(out=st[:, hf, :], in_=y[:, hf * 512:(hf + 1) * 512]),
             reads=[key_y], writes=["sm_ln"])
    p.op("dve", lambda e: e.bn_aggr(out=sm[:, 12:14], in_=sm[:, 0:12]), reads=["sm_ln"], writes=["sm_ln"])
    p.act(sm[:, 14:15], sm[:, 13:14], AF.Sqrt, bias=sm[:, 20:21], reads=["sm_ln", "sm_eps"], writes=["sm_ln"])
    p.op("dve", lambda e: e.reciprocal(out=sm[:, 15:16], in_=sm[:, 14:15]), reads=["sm_ln"], writes=["sm_ln"])
    p.ts("dve", sm[:, 16:17], sm[:, 12:13], sm[:, 15:16], -1.0, ALU.mult, ALU.mult,
         reads=["sm_ln"], writes=["sm_ln"])
    p.act(out, y, AF.Identity, bias=sm[:, 16:17], scale=sm[:, 15:16], reads=[key_y, "sm_ln"], writes=[key_out])
    p.tt("dve", out, out, gbc, ALU.mult, reads=[key_out] + list(extra_reads), writes=[key_out])
    p.tt("dve", out, out, bbc, ALU.add, reads=[key_out], writes=[key_out])


def build(stop_after=99, stop_phase=7):
    nc = bass.Bass("TRN2", target_bir_lowering=False)
    p = Prog(nc)
    din = lambda n, s: nc.dram_tensor(n, list(s), F32, kind="ExternalInput").ap()
    hin = din("hin", [R, D])
    w_in = din("w_in", [D, 3080])
    bgate = din("bgate", [4, 2])
    norm_pk = din("norm_pk", [128, 8])
    w_out = din("w_out", [D, D])
    lng = din("lng", [4, D])
    lnb = din("lnb", [4, D])
    consts = din("consts", [128, 1024])
    consts2 = din("consts2", [128, 4096])
    w_gd = din("w_gd", [D, DFF])
    w_ud = din("w_ud", [D, DFF])
    w_dd = din("w_dd", [DFF, D])
    w_kv = din("w_kv", [D, 2 * D])
    w_q = din("w_q", [D, D])
    w_o = din("w_o", [D, D])
    w_r = din("w_r", [D, NE])
    w_ge = din("w_ge", [NE, D, DFE])
    w_ue = din("w_ue", [NE, D, DFE])
    w_de = din("w_de", [NE, DFE, D])
    out_d = nc.dram_tensor("out", [R, D], F32, kind="ExternalOutput").ap()
    h_a = p.dram("h_a", [R, D], F32)
    h_b = p.dram("h_b", [R, D], F32)
    h_c = p.dram("h_c", [R, D], F32)
    KT_d = p.dram("KT_d", [8, 128, R], BF16)
    QT_d = p.dram("QT_d", [8, 128, R], BF16)
    V_d = p.dram("V_d", [R, D], BF16)
    OT_d = p.dram("OT_d", [8, 128, R], BF16)
    NSLOT = NE * CAP
    Xslot = p.dram("Xslot", [NSLOT + 128, D], BF16)
    Yslot = p.dram("Yslot", [NSLOT + 128, D], F32)

    cst = p.sb("cst", [128, 1024], F32)
    p.ld("sp", cst[:], consts, writes=["cst"])
    ident_bf = p.sb("ident_bf", [128, 128], BF16)
    p.ld("pool", ident_bf[:], consts[:, 0:128], writes=["ident_bf"])
    ones_bf = p.sb("ones_bf", [128, 8], BF16)
    p.op("dve", lambda e: e.memset(ones_bf[:], 1.0), writes=["ones_bf"])
    ident_f = cst[:, 0:128]
    cmask = cst[:, 128:256]
    sel = cst[0:4, 256:768].rearrange("p (h n) -> p h n", h=4)
    bg = p.sb("bg", [4, 4], F32)
    p.ld("sp", bg[:, 0:2], bgate, writes=["bg"])
    p.ts("dve", bg[:, 2:3], bg[:, 1:2], -1.0, None, ALU.mult, reads=["bg"], writes=["bg"])
    p.op("dve", lambda e: e.memset(bg[:, 3:4], 0.0), reads=["bg"], writes=["bg"])
    ones4 = p.sb("ones4", [4, 128], F32)
    p.op("dve", lambda e: e.memset(ones4[:], 1.0), writes=["ones4"])
    sm = p.sb("sm", [128, 32], F32)
    p.op("dve", lambda e: e.memset(sm[:, 20:21], EPS), writes=["sm_eps"])
    gbc = p.sb("gbc", [128, D], F32)
    bbc = p.sb("bbc", [128, D], F32)

    tpb = p.ps("tpb", [128, 8, 128], BF16)
    PB = [p.ps("pb%d" % i, [128, 512], F32) for i in range(7)]
    pA, pB, pC, pD, pE, pF, pG = PB

    p.phase_begin()
    if True:
        p.ld("sp", gbc[:], lng[0:1, :].partition_broadcast(128), writes=["gbc"])
        p.ld("sp", bbc[:], lnb[0:1, :].partition_broadcast(128), writes=["bbc"])
        win = load_w_bf(p, "win", w_in, D, 3080, "win")
        wout = load_w_bf(p, "wout", w_out, D, D, "wout")
        npk = p.sb("npk", [128, 8], F32)
        p.ld("sp", npk[:], norm_pk, writes=["npk"])
        for kc in range(8):
            p.ts("pool", wout[:, kc, :], wout[:, kc, :], npk[:, kc:kc + 1], None, ALU.mult,
                 reads=["wout", "npk"], writes=["wout"])
        xbf = [p.sb("xbf%d" % i, [128, D], BF16) for i in range(2)]
        xf = [p.sb("xf%d" % i, [128, D], F32) for i in range(2)]
        hT = [p.sb("hT%d" % i, [128, 8, 128], BF16) for i in range(2)]
        G = [p.sb("G%d" % i, [4, 12, 128], F32) for i in range(2)]
        Wbc = p.sb("Wbc", [128, 4, 128], F32)
        tok = p.sb("tok", [128, 12], F32)
        qT = p.sb("qT", [128, 4, 128], BF16)
        kT = p.sb("kT", [128, 4, 128], BF16)
        wk = p.sb("wk", [128, 4, 128], BF16)
        V = p.sb("V", [128, 4, 256], BF16)
        osig = p.sb("osig", [128, D], BF16)
        PT = p.sb("PT", [128, 4, 128], BF16)
        Cf = p.sb("Cf", [128, 4, 256], F32)
        nf = p.sb("nf", [128, 4], F32)
        Cbf = p.sb("Cbf", [128, 4, 256], BF16)
        nbf = p.sb("nbf", [128, 4], BF16)
        nrm = p.sb("nrm", [128, 64], F32)
        hn = p.sb("hn", [128, D], F32)
        outtok = p.sb("outtok", [128, D], BF16)
        outT = p.sb("outT", [128, 8, 128], BF16)
        y = p.sb("y", [128, D], F32)
        ha = [p.sb("ha%d" % i, [128, D], F32) for i in range(2)]
        KSC = 128.0 ** -0.5

        def stage_load(t):
            sl = t % 2
            rows = slice(t * 128, (t + 1) * 128)
            p.ld("pool", xbf[sl][:], hin[rows, :], writes=["xbf%d" % sl])
            p.ld("sp", xf[sl][:], hin[rows, :], writes=["xf%d" % sl])

        stage_load(0)
        for t in range(NT):
            if t >= stop_after:
                break
            sl = t % 2
            c = t % TPS
            first = c == 0
            if t + 1 < NT:
                stage_load(t + 1)
            g = G[sl]
            gp = G[1 - sl]
            gk = "G%d" % sl
            gpk = "G%d" % (1 - sl)
            for kc in range(8):
                p.tr(tpb[:, kc, :], xbf[sl][:, kc * 128:(kc + 1) * 128], ident_bf[:],
                     reads=["xbf%d" % sl, "ident_bf"], writes=["tpb"], signal=(kc == 7))
            p.cp("act", hT[sl][:], tpb[:], reads=["tpb"], writes=["hT%d" % sl])
            hk = "hT%d" % sl
            for gi in range(2):
                for kc in range(8):
                    p.mm(pE[0:4, gi * 128:(gi + 1) * 128], win[:, kc, 3072 + 4 * gi:3076 + 4 * gi],
                         hT[sl][:, kc, :], start=(kc == 0), stop=(kc == 7),
                         reads=[hk, "win"], writes=["pE_g"], signal=(kc == 7))
            p.act(g[:, 0, :], pE[0:4, 0:128], AF.Identity, bias=bg[:, 0:1], reads=["pE_g", "bg"], writes=[gk])
            p.act(g[:, 10, :], pE[0:4, 128:256], AF.Exp, bias=bg[:, 2:3], scale=-1.0,
                  reads=["pE_g", "bg"], writes=[gk])
            p.act(g[:, 11, :], g[:, 10, :], AF.Ln, bias=1.0, reads=[gk], writes=[gk])
            p.ts("dve", g[:, 1, :], g[:, 11, :], -1.0, None, ALU.mult, reads=[gk], writes=[gk])
            if first:
                p.op("dve", lambda e, g=g: e.memset(g[:, 0, 0:PAD], -30000.0), reads=[gk], writes=[gk])
                p.op("dve", lambda e, g=g: e.memset(g[:, 1, 0:PAD], 0.0), reads=[gk], writes=[gk])
                Bi, mi, a0 = bg[:, 3:4], bg[:, 3:4], bg[:, 3:4]
            else:
                Bi, mi, a0 = gp[:, 2, 127:128], gp[:, 3, 127:128], gp[:, 4, 127:128]
            p.op("dve", lambda e, g=g, Bi=Bi: e.tensor_tensor_scan(
                out=g[:, 2, :], data0=ones4[:], data1=g[:, 1, :], initial=Bi, op0=ALU.mult, op1=ALU.add),
                reads=[gk, gpk, "ones4", "bg"], writes=[gk])
            p.op("dve", lambda e, g=g, mi=mi: e.tensor_tensor_scan(
                out=g[:, 3, :], data0=g[:, 1, :], data1=g[:, 0, :], initial=mi, op0=ALU.add, op1=ALU.max),
                reads=[gk, gpk, "bg"], writes=[gk])
            p.tt("dve", g[:, 4, :], g[:, 2, :], g[:, 3, :], ALU.subtract, reads=[gk], writes=[gk])
            p.tt("dve", g[:, 6, :], g[:, 0, :], g[:, 2, :], ALU.subtract, reads=[gk], writes=[gk])
            p.ts("dve", g[:, 10, :], g[:, 4, :], a0, None, ALU.subtract, reads=[gk, gpk, "bg"], writes=[gk])
            p.act(g[:, 5, :], g[:, 10, :], AF.Exp, reads=[gk], writes=[gk])
            p.act(g[:, 7, :], g[:, 6, :], AF.Exp, bias=a0, reads=[gk, gpk, "bg"], writes=[gk])
            p.act(g[:, 8, :], g[:, 6, :], AF.Exp, bias=g[:, 4, 127:128], reads=[gk], writes=[gk])
            p.act(g[:, 9, :], g[:, 3, :], AF.Exp, scale=-1.0, reads=[gk], writes=[gk])
            for h in range(4):
                p.mm(pD[:, h * 128:(h + 1) * 128], sel[:, h, :], g[:, 5, :], reads=[gk, "cst"],
                     writes=["pD"], signal=(h == 3))
            p.cp("act", Wbc[:].rearrange("p h n -> p (h n)"), pD[:], reads=["pD"], writes=["Wbc"])
            for j in range(3):
                p.tr(pE[:, 256 + 4 * j:260 + 4 * j], g[:, 7 + j, :], ident_f[0:4, 0:4],
                     reads=[gk, "cst"], writes=["pE_t"], signal=(j == 2))
            p.cp("dve", tok[:], pE[:, 256:268], reads=["pE_t"], writes=["tok"])
            for h in range(4):
                for kc in range(8):
                    p.mm(pA[:, h * 128:(h + 1) * 128], win[:, kc, h * 128:(h + 1) * 128], hT[sl][:, kc, :],
                         start=(kc == 0), stop=(kc == 7), reads=[hk, "win"], writes=["pA"],
                         signal=(h == 3 and kc == 7))
            p.tt("dve", qT[:].rearrange("p h n -> p (h n)"), pA[:], Wbc[:].rearrange("p h n -> p (h n)"),
                 ALU.mult, reads=["pA", "Wbc"], writes=["qT"])
            for h in range(4):
                for kc in range(8):
                    p.mm(pA[:, h * 128:(h + 1) * 128], win[:, kc, 512 + h * 128:512 + (h + 1) * 128],
                         hT[sl][:, kc, :], start=(kc == 0), stop=(kc == 7), reads=[hk, "win"], writes=["pA"],
                         signal=(h == 3 and kc == 7))
            p.op("act", lambda e: e.mul(out=kT[:].rearrange("p h n -> p (h n)"), in_=pA[:], mul=KSC),
                 reads=["pA"], writes=["kT"])
            banks = [pB, pC]
            bk = ["pB", "pC"]

            def tokmm(bi, col0):
                for kc in range(8):
                    p.mm(banks[bi][:], hT[sl][:, kc, :], win[:, kc, col0:col0 + 512], start=(kc == 0),
                         stop=(kc == 7), reads=[hk, "win"], writes=[bk[bi]], signal=(kc == 7))

            tokmm(0, 512)
            for h in range(4):
                p.ts("dve", wk[:, h, :], pB[:, h * 128:(h + 1) * 128], tok[:, 4 + h:5 + h], KSC,
                     ALU.mult, ALU.mult, reads=["pB", "tok"], writes=["wk"])
            tokmm(1, 1024)
            p.cp("act", V[:, 0:2, :].rearrange("p h n -> p (h n)"), pC[:], reads=["pC"], writes=["V"])
            tokmm(0, 1536)
            p.cp("act", V[:, 2:4, :].rearrange("p h n -> p (h n)"), pB[:], reads=["pB"], writes=["V"])
            tokmm(1, 2048)
            p.act(osig[:, 0:512], pC[:], AF.Sigmoid, reads=["pC"], writes=["osig"])
            tokmm(0, 2560)
            p.act(osig[:, 512:1024], pB[:], AF.Sigmoid, reads=["pB"], writes=["osig"])
            if first:
                p.op("dve", lambda e: e.memset(Cf[:], 0.0), writes=["Cf"])
                p.op("dve", lambda e: e.memset(nf[:], 0.0), writes=["nf"])
                p.op("dve", lambda e: e.memset(Cbf[:], 0.0), writes=["Cbf"])
                p.op("dve", lambda e: e.memset(nbf[:], 0.0), writes=["nbf"])
            for h in range(4):
                p.mm(pA[:, h * 128:(h + 1) * 128], kT[:, h, :], qT[:, h, :], reads=["kT", "qT"], writes=["pA"],
                     signal=(h == 3))
            for h in range(4):
                p.stt(PT[:, h, :], pA[:, h * 128:(h + 1) * 128], tok[:, h:h + 1], cmask, ALU.mult, ALU.mult,
                      reads=["pA", "tok", "cst"], writes=["PT"])
            for h in range(4):
                bank = pF if h < 2 else pG
                bkey = "pF" if h < 2 else "pG"
                reg = bank[:, (h % 2) * 256:(h % 2) * 256 + 256]
                p.mm(reg, PT[:, h, :], V[:, h, :], start=True, stop=False, reads=["PT", "V"], writes=[bkey],
                     signal=False)
                p.mm(reg, qT[:, h, :], Cbf[:, h, :], start=False, stop=True, reads=["qT", "Cbf"], writes=[bkey])
                p.mm(pE[:, 272 + h:273 + h], PT[:, h, :], ones_bf[:, 0:1], start=True, stop=False,
                     reads=["PT", "ones_bf"], writes=["pE_d"], signal=False)
                p.mm(pE[:, 272 + h:273 + h], qT[:, h, :], nbf[:, h:h + 1], start=False, stop=True,
                     reads=["qT", "nbf"], writes=["pE_d"])
            p.act(nrm[:, 0:4], pE[:, 272:276], AF.Abs, reads=["pE_d"], writes=["nrm"])
            p.tt("dve", nrm[:, 0:4], nrm[:, 0:4], tok[:, 8:12], ALU.max, reads=["nrm", "tok"], writes=["nrm"])
            p.op("dve", lambda e: e.reciprocal(out=nrm[:, 0:4], in_=nrm[:, 0:4]), reads=["nrm"], writes=["nrm"])
            for h in range(4):
                bank = pF if h < 2 else pG
                bkey = "pF" if h < 2 else "pG"
                reg = bank[:, (h % 2) * 256:(h % 2) * 256 + 256]
                p.op("dve", lambda e, h=h, reg=reg: e.bn_stats(out=nrm[:, 4 + 6 * h:10 + 6 * h], in_=reg),
                     reads=[bkey, "nrm"], writes=["nrm"])
                p.op("dve", lambda e, h=h: e.bn_aggr(out=nrm[:, 28 + 2 * h:30 + 2 * h],
                                                     in_=nrm[:, 4 + 6 * h:10 + 6 * h]),
                     reads=["nrm"], writes=["nrm"])
            mv = nrm[:, 28:36].rearrange("p (h two) -> p h two", two=2)
            p.tt("dve", nrm[:, 36:40], nrm[:, 0:4], nrm[:, 0:4], ALU.mult, reads=["nrm"], writes=["nrm"])
            p.tt("dve", nrm[:, 36:40], nrm[:, 36:40], mv[:, :, 1], ALU.mult, reads=["nrm"], writes=["nrm"])
            p.act(nrm[:, 36:40], nrm[:, 36:40], AF.Sqrt, bias=sm[:, 20:21], reads=["nrm", "sm_eps"],
                  writes=["nrm"])
            p.op("dve", lambda e: e.reciprocal(out=nrm[:, 36:40], in_=nrm[:, 36:40]), reads=["nrm"],
                 writes=["nrm"])
            p.tt("dve", nrm[:, 40:44], nrm[:, 36:40], nrm[:, 0:4], ALU.mult, reads=["nrm"], writes=["nrm"])
            p.tt("dve", nrm[:, 44:48], nrm[:, 40:44], mv[:, :, 0], ALU.mult, reads=["nrm"], writes=["nrm"])
            p.ts("dve", nrm[:, 44:48], nrm[:, 44:48], -1.0, None, ALU.mult, reads=["nrm"], writes=["nrm"])
            for h in range(4):
                bank = pF if h < 2 else pG
                bkey = "pF" if h < 2 else "pG"
                reg = bank[:, (h % 2) * 256:(h % 2) * 256 + 256]
                p.act(hn[:, h * 256:(h + 1) * 256], reg, AF.Identity, bias=nrm[:, 44 + h:45 + h],
                      scale=nrm[:, 40 + h:41 + h], reads=[bkey, "nrm"], writes=["hn"])
            p.tt("dve", outtok[:], hn[:], osig[:], ALU.mult, reads=["hn", "osig"], writes=["outtok"])
            for h in range(4):
                bank = pF if h < 2 else pG
                bkey = "pF" if h < 2 else "pG"
                reg = bank[:, (h % 2) * 256:(h % 2) * 256 + 256]
                p.mm(reg, wk[:, h, :], V[:, h, :], reads=["wk", "V"], writes=[bkey])
                p.mm(pE[:, 280 + h:281 + h], wk[:, h, :], ones_bf[:, 0:1], reads=["wk", "ones_bf"],
                     writes=["pE_n"])
                dec = Wbc[:, h, 127:128]
                p.stt(Cf[:, h, :], Cf[:, h, :], dec, reg, ALU.mult, ALU.add, reads=["Cf", "Wbc", bkey],
                      writes=["Cf"])
                p.stt(nf[:, h:h + 1], nf[:, h:h + 1], dec, pE[:, 280 + h:281 + h], ALU.mult, ALU.add,
                      reads=["nf", "Wbc", "pE_n"], writes=["nf"])
            p.cp("act", Cbf[:].rearrange("p h n -> p (h n)"), Cf[:].rearrange("p h n -> p (h n)"),
                 reads=["Cf"], writes=["Cbf"])
            p.cp("act", nbf[:], nf[:], reads=["nf"], writes=["nbf"])
            for kc in range(8):
                p.tr(tpb[:, kc, :], outtok[:, kc * 128:(kc + 1) * 128], ident_bf[:],
                     reads=["outtok", "ident_bf"], writes=["tpb"], signal=(kc == 7))
            p.cp("act", outT[:], tpb[:], reads=["tpb"], writes=["outT"])
            for hf in range(2):
                bi = 1 - hf
                for kc in range(8):
                    p.mm(banks[bi][:], outT[:, kc, :], wout[:, kc, hf * 512:(hf + 1) * 512], start=(kc == 0),
                         stop=(kc == 7), reads=["outT", "wout"], writes=[bk[bi]], signal=(kc == 7))
                p.stt(y[:, hf * 512:(hf + 1) * 512], xf[sl][:, hf * 512:(hf + 1) * 512], ALPHA, banks[bi][:],
                      ALU.mult, ALU.add, reads=["xf%d" % sl, bk[bi]], writes=["y"])
            layernorm_tile(p, y[:], ha[sl][:], gbc[:], bbc[:], sm, "y", "ha%d" % sl, extra_reads=["gbc", "bbc"])
            p.ld("sp", h_a[t * 128:(t + 1) * 128, :], ha[sl][:], reads=["ha%d" % sl], writes=["h_a_d%d" % t])

    p.phase_end()
    dbg_src = h_a
    n_dbg = min(NT, stop_after)

    def load_c2(p):
        c2 = p.sb("c2", [128, 4096], F32)
        p.ld("sp", c2[:], consts2, writes=["c2"])
        return c2

    def transpose_rows(p, src_bf, dst, col0, rk, wk_):
        for kc in range(8):
            p.tr(tpb[:, kc, :], src_bf[:, kc * 128:(kc + 1) * 128], ident_bf[:],
                 reads=[rk, "ident_bf"], writes=["tpb"], signal=(kc == 7))
        p.cp("act", dst[:, :, col0:col0 + 128], tpb[:], reads=["tpb"], writes=[wk_])

    if stop_phase >= 2:
        p.phase_begin()
        p.ld("sp", gbc[:], lng[1:2, :].partition_broadcast(128), writes=["gbc"])
        p.ld("sp", bbc[:], lnb[1:2, :].partition_broadcast(128), writes=["bbc"])
        wg = load_w_bf(p, "wg", w_gd, D, DFF, "wg")
        wu = load_w_bf(p, "wu", w_ud, D, DFF, "wu")
        wd = load_w_bf(p, "wd", w_dd, DFF, D, "wd")
        NF = DFF // 128
        xbf2 = [p.sb("x2bf%d" % i, [128, 2, D], BF16) for i in range(2)]
        xf2 = [p.sb("x2f%d" % i, [128, D], F32) for i in range(2)]
        hT2 = p.sb("hT2", [128, 8, 256], BF16)
        HT = p.sb("HT", [128, NF, 256], BF16)
        sg = [p.sb("sg%d" % i, [128, 256], F32) for i in range(2)]
        y2 = p.sb("y2", [128, D], F32)
        hb = [p.sb("hb%d" % i, [128, D], F32) for i in range(2)]
        NG = R // 256

        def ld2(g):
            sl = g % 2
            p.ld("pool", xbf2[sl][:], h_a[g * 256:(g + 1) * 256, :].rearrange("(j p) d -> p j d", p=128),
                 reads=["h_a_d%d" % (2 * g), "h_a_d%d" % (2 * g + 1)], writes=["x2bf%d" % sl])

        ld2(0)
        for g in range(NG):
            sl = g % 2
            if g + 1 < NG:
                ld2(g + 1)
            for j in range(2):
                transpose_rows(p, xbf2[sl][:, j, :], hT2, j * 128, "x2bf%d" % sl, "hT2")
            for f in range(NF):
                gb, ub = PB[f % 2], PB[2 + f % 2]
                gk_, uk_ = "pb%d" % (f % 2), "pb%d" % (2 + f % 2)
                for kc in range(8):
                    p.mm(gb[:, 0:256], wg[:, kc, f * 128:(f + 1) * 128], hT2[:, kc, :], start=(kc == 0),
                         stop=(kc == 7), reads=["hT2", "wg"], writes=[gk_], signal=(kc == 7))
                for kc in range(8):
                    p.mm(ub[:, 0:256], wu[:, kc, f * 128:(f + 1) * 128], hT2[:, kc, :], start=(kc == 0),
                         stop=(kc == 7), reads=["hT2", "wu"], writes=[uk_], signal=(kc == 7))
                p.act(sg[f % 2][:], gb[:, 0:256], AF.Silu, reads=[gk_], writes=["sg%d" % (f % 2)])
                p.tt("dve", HT[:, f, :], sg[f % 2][:], ub[:, 0:256], ALU.mult, reads=["sg%d" % (f % 2), uk_],
                     writes=["HT"])
            for j in range(2):
                t = 2 * g + j
                s2 = t % 2
                p.ld("sp", xf2[s2][:], h_a[t * 128:(t + 1) * 128, :], reads=["h_a_d%d" % t],
                     writes=["x2f%d" % s2])
                for hf in range(2):
                    bank = PB[4 + hf]
                    bkey = "pb%d" % (4 + hf)
                    for f in range(NF):
                        p.mm(bank[:], HT[:, f, j * 128:(j + 1) * 128], wd[:, f, hf * 512:(hf + 1) * 512],
                             start=(f == 0), stop=(f == NF - 1), reads=["HT", "wd"], writes=[bkey],
                             signal=(f == NF - 1))
                    p.stt(y2[:, hf * 512:(hf + 1) * 512], xf2[s2][:, hf * 512:(hf + 1) * 512], ALPHA, bank[:],
                          ALU.mult, ALU.add, reads=["x2f%d" % s2, bkey], writes=["y2"])
                layernorm_tile(p, y2[:], hb[s2][:], gbc[:], bbc[:], sm, "y2", "hb%d" % s2,
                               extra_reads=["gbc", "bbc"])
                p.ld("sp", h_b[t * 128:(t + 1) * 128, :], hb[s2][:], reads=["hb%d" % s2], writes=["h_b_d%d" % t])
        p.phase_end()
        dbg_src = h_b
        n_dbg = NT

    if stop_phase >= 3:
        p.phase_begin()
        wkv = load_w_bf(p, "wkv", w_kv, D, 2 * D, "wkv")
        wq = load_w_bf(p, "wq", w_q, D, D, "wq")
        zt = p.sb("zt", [128, 8192], BF16)
        p.op("pool", lambda e: e.memset(zt[:], 0.0), writes=["zt"])
        zrows = 128 * 8
        for i in range(NSLOT // zrows):
            p.ld("sp", Xslot[i * zrows:(i + 1) * zrows, :].rearrange("(p j) d -> p (j d)", p=128), zt[:],
                 reads=["zt"], writes=["Xslot"])
        zf = p.sb("zf", [128, D], F32)
        p.op("pool", lambda e: e.memset(zf[:], 0.0), writes=["zf"])
        p.ld("sp", Yslot[NSLOT:NSLOT + 128, :], zf[:], reads=["zf"], writes=["Yzero"])
        xbf3 = [p.sb("x3bf%d" % i, [128, 4, D], BF16) for i in range(2)]
        hT3 = p.sb("hT3", [128, 8, 512], BF16)
        kq = [p.sb("kq%d" % i, [128, 512], BF16) for i in range(2)]
        vs = [p.sb("vs%d" % i, [128, D], BF16) for i in range(2)]
        QSC = 128.0 ** -0.5
        groups = [(g * 512, 512) for g in range(R // 512)]
        if R % 512:
            groups.append((R - R % 512, R % 512))

        def ld3(gi):
            r0, w = groups[gi]
            sl = gi % 2
            nj = w // 128
            p.ld("pool", xbf3[sl][:, 0:nj, :], h_b[r0:r0 + w, :].rearrange("(j p) d -> p j d", p=128),
                 reads=["h_b_d%d" % (r0 // 128 + j) for j in range(nj)], writes=["x3bf%d" % sl])

        ld3(0)
        cnt3 = 0
        for gi, (r0, w) in enumerate(groups):
            sl = gi % 2
            nj = w // 128
            if gi + 1 < len(groups):
                ld3(gi + 1)
            for j in range(nj):
                transpose_rows(p, xbf3[sl][:, j, :], hT3, j * 128, "x3bf%d" % sl, "hT3")
            for which in range(2):
                for h in range(8):
                    bank = PB[cnt3 % 4]
                    bkey = "pb%d" % (cnt3 % 4)
                    wsrc = wkv if which == 0 else wq
                    for kc in range(8):
                        p.mm(bank[:, 0:w], wsrc[:, kc, h * 128:(h + 1) * 128], hT3[:, kc, 0:w], start=(kc == 0),
                             stop=(kc == 7), reads=["hT3", "wkv", "wq"], writes=[bkey], signal=(kc == 7))
                    ks = kq[cnt3 % 2]
                    kk = "kq%d" % (cnt3 % 2)
                    if which == 0:
                        p.cp("act", ks[:, 0:w], bank[:, 0:w], reads=[bkey], writes=[kk])
                        p.ld("sp", KT_d[h, :, r0:r0 + w], ks[:, 0:w], reads=[kk], writes=["KT_d"])
                    else:
                        p.op("act", lambda e, ks=ks, bank=bank, w=w: e.mul(out=ks[:, 0:w], in_=bank[:, 0:w],
                                                                             mul=QSC), reads=[bkey], writes=[kk])
                        p.ld("sp", QT_d[h, :, r0:r0 + w], ks[:, 0:w], reads=[kk], writes=["QT_d"])
                    cnt3 += 1
            for j in range(nj):
                t = r0 // 128 + j
                v_ = vs[t % 2]
                vk = "vs%d" % (t % 2)
                for hf in range(2):
                    bank = PB[4 + hf]
                    bkey = "pb%d" % (4 + hf)
                    for kc in range(8):
                        p.mm(bank[:], hT3[:, kc, j * 128:(j + 1) * 128], wkv[:, kc, D + hf * 512:D + (hf + 1) * 512],
                             start=(kc == 0), stop=(kc == 7), reads=["hT3", "wkv"], writes=[bkey],
                             signal=(kc == 7))
                    if hf == 0:
                        p.cp("dve", v_[:, 0:512], bank[:], reads=[bkey], writes=[vk])
                    else:
                        p.cp("act", v_[:, 512:1024], bank[:], reads=[bkey], writes=[vk])
                p.ld("sp", V_d[t * 128:(t + 1) * 128, :], v_[:], reads=[vk], writes=["V_d"])
        p.phase_end()

    if stop_phase >= 4:
        p.phase_begin()
        c2 = load_c2(p)
        Ub = p.sb("Ub", [128, 128], BF16)
        Lb = p.sb("Lb", [128, 128], BF16)
        p.cp("dve", Ub[:], c2[:, 2048:2176], reads=["c2"], writes=["Ub"])
        p.cp("dve", Lb[:], cst[:, 128:256], reads=["cst"], writes=["Lb"])
        rowmask = c2[:, 2432:2433]
        mbf = p.sb("mbf", [128, 2048], BF16)
        p.cp("dve", mbf[:], c2[:, 0:2048], reads=["c2"], writes=["mbf"])
        negm = p.sb("negm", [128, 2048], F32)
        p.ts("dve", negm[:], c2[:, 0:2048], 30000.0, -30000.0, ALU.mult, ALU.add, reads=["c2"], writes=["negm"])
        negrow = p.sb("negrow", [128, 2], F32)
        p.ts("dve", negrow[:, 0:1], rowmask, 30000.0, -30000.0, ALU.mult, ALU.add, reads=["c2"], writes=["negrow"])
        p.op("dve", lambda e: e.memset(negrow[:, 1:2], 0.0), reads=["negrow"], writes=["negrow"])
        KTs = [p.sb("KTs%d" % i, [128, RS], BF16) for i in range(4)]
        QTs = [p.sb("QTs%d" % i, [128, RS], BF16) for i in range(4)]
        Vbs = [p.sb("Vbs%d" % i, [128, TPS, 128], BF16) for i in range(4)]
        Eb = [p.sb("Eb%d" % i, [128, 512], F32) for i in range(2)]
        SPB = [p.sb("SPB%d" % i, [128, 512], BF16) for i in range(4)]
        Tb = [p.sb("Tb%d" % i, [128, 512], F32) for i in range(4)]
        AB = [p.sb("AB%d" % i, [128, 512], BF16) for i in range(4)]
        OTs = [p.sb("OTs%d" % i, [128, 512], BF16) for i in range(2)]

        def ld4(h):
            for s_ in range(2):
                sl = (h % 2) * 2 + s_
                rr = slice(s_ * RS, (s_ + 1) * RS)
                p.ld("sp", KTs[sl][:], KT_d[h, :, rr], writes=["KTs%d" % sl])
                p.ld("sp", QTs[sl][:], QT_d[h, :, rr], writes=["QTs%d" % sl])
                p.ld("sp", Vbs[sl][:], V_d[rr, h * 128:(h + 1) * 128].rearrange("(j p) d -> p j d", p=128),
                     writes=["Vbs%d" % sl])

        steps = []
        first_x = {}
        for h in range(8):
            for qi in range(9):
                jmax = min(4 * qi + 3, TPS - 1)
                for j in range(jmax, -1, -1):
                    for s_ in range(2):
                        first_x.setdefault(h, len(steps))
                        steps.append(dict(s=s_, h=h, qi=qi, j=j, jmax=jmax, x=len(steps),
                                          sl=(h % 2) * 2 + s_))
        NS = len(steps)
        Zb = [PB[0], PB[1], PB[0]]
        zkeys = ["pb0", "pb1", "pb0"]
        NDUMMY = 2

        def geo(st):
            q0 = st["qi"] * 512
            W = min(512, RS - q0)
            return q0, W, st["j"] - 4 * st["qi"]

        def stage1(st):
            x, sl, j = st["x"], st["sl"], st["j"]
            q0, W, r = geo(st)
            if x == first_x[st["h"]] + 4 and st["h"] + 1 < 8:
                ld4(st["h"] + 1)
            Z, zk = Zb[x % 2], zkeys[x % 2]
            b2, b3 = x % 2, x % 4
            p.mm(Z[:, 0:W], KTs[sl][:, j * 128:(j + 1) * 128], QTs[sl][:, q0:q0 + W],
                 reads=["KTs%d" % sl, "QTs%d" % sl], writes=[zk])
            p.act(Eb[b2][:, 0:W], Z[:, 0:W], AF.Exp, reads=[zk], writes=["Eb%d" % b2])
            p.act(SPB[b3][:, 0:W], Eb[b2][:, 0:W], AF.Ln, bias=1.0, reads=["Eb%d" % b2], writes=["SPB%d" % b3])

        def stage1b(st):
            x, j = st["x"], st["j"]
            q0, W, r = geo(st)
            Z, zk = Zb[x % 2], zkeys[x % 2]
            b2, b3 = x % 2, x % 4
            if r >= 0:
                p.tt("dve", SPB[b3][:, 0:W], SPB[b3][:, 0:W], mbf[:, r * 512:r * 512 + W], ALU.mult,
                     reads=["SPB%d" % b3, "mbf"], writes=["SPB%d" % b3])
            if j == 0:
                p.ts("dve", SPB[b3][:, 0:W], SPB[b3][:, 0:W], rowmask, None, ALU.mult,
                     reads=["SPB%d" % b3, "c2"], writes=["SPB%d" % b3])
            p.tt("dve", Tb[b3][:, 0:W], Z[:, 0:W], SPB[b3][:, 0:W], ALU.subtract, reads=[zk, "SPB%d" % b3],
                 writes=["Tb%d" % b3])
            if r >= 0:
                p.tt("pool", Tb[b3][:, 0:W], Tb[b3][:, 0:W], negm[:, r * 512:r * 512 + W], ALU.add,
                     reads=["Tb%d" % b3, "negm"], writes=["Tb%d" % b3])

        def stage2a(st):
            x, j = st["x"], st["j"]
            q0, W, r = geo(st)
            b3 = x % 4
            A, ak = PB[2 + st["s"]], "pb%d" % (2 + st["s"])
            p.mm(A[:, 0:W], Ub[:], SPB[b3][:, 0:W], start=(j == st["jmax"]), stop=False,
                 reads=["Ub", "SPB%d" % b3], writes=[ak])
            p.tt("dve", Tb[b3][:, 0:W], Tb[b3][:, 0:W], A[:, 0:W], ALU.subtract, reads=["Tb%d" % b3, ak],
                 writes=["Tb%d" % b3])

        def stage2b(st):
            x, j = st["x"], st["j"]
            q0, W, r = geo(st)
            b3 = x % 4
            A, ak = PB[2 + st["s"]], "pb%d" % (2 + st["s"])
            p.mm(A[:, 0:W], Lb[:], SPB[b3][:, 0:W], start=False, stop=(j == 0),
                 reads=["Lb", "SPB%d" % b3], writes=[ak])

        def stage2c(st):
            x, j = st["x"], st["j"]
            q0, W, r = geo(st)
            b3 = x % 4
            p.act(AB[b3][:, 0:W], Tb[b3][:, 0:W], AF.Exp, bias=(negrow[:, 0:1] if j == 0 else negrow[:, 1:2]),
                  reads=["Tb%d" % b3, "negrow"], writes=["AB%d" % b3])

        def stage3(st):
            x, sl, j = st["x"], st["sl"], st["j"]
            q0, W, r = geo(st)
            b3 = x % 4
            O, ok_ = PB[4 + st["s"]], "pb%d" % (4 + st["s"])
            p.mm(O[:, 0:W], Vbs[sl][:, j, :], AB[b3][:, 0:W], start=(j == st["jmax"]), stop=(j == 0),
                 reads=["Vbs%d" % sl, "AB%d" % b3], writes=[ok_])
            if j == 0:
                ot, otk = OTs[st["s"]], "OTs%d" % st["s"]
                p.cp("act", ot[:, 0:W], O[:, 0:W], reads=[ok_], writes=[otk])
                p.ld("sp", OT_d[st["h"], :, st["s"] * RS + q0:st["s"] * RS + q0 + W], ot[:, 0:W], reads=[otk],
                     writes=["OT_d"])

        ld4(0)
        for n in range(NS + 3):
            if 0 <= n - 2 < NS:
                stage2b(steps[n - 2])
            if 0 <= n - 1 < NS:
                stage2a(steps[n - 1])
            if n < NS:
                stage1(steps[n])
            for _ in range(NDUMMY):
                p.mm(PB[6][:, 0:512], Ub[:], mbf[:, 0:512], reads=["Ub", "mbf"], writes=["pb6"], signal=False)
            if 0 <= n - 3 < NS:
                stage3(steps[n - 3])
            if 0 <= n - 2 < NS:
                stage2c(steps[n - 2])
            if n < NS:
                stage1b(steps[n])
        p.phase_end()

    rinfo = p.sb("rinfo", [128, NT, 4], F32)
    rpos = p.sb("rpos", [128, NT, 2], I32)
    if stop_phase >= 5:
        p.phase_begin()
        c2 = load_c2(p)
        p.ld("sp", gbc[:], lng[2:3, :].partition_broadcast(128), writes=["gbc"])
        p.ld("sp", bbc[:], lnb[2:3, :].partition_broadcast(128), writes=["bbc"])
        wo = load_w_bf(p, "wo", w_o, D, D, "wo")
        wr = p.sb("wr", [128, 8, NE], F32)
        p.ld("sp", wr[:], w_r.rearrange("(k p) e -> p k e", p=128), writes=["wr"])
        SLb = p.sb("SLb", [128, 128], BF16)
        UIb = p.sb("UIb", [128, 128], BF16)
        p.cp("dve", SLb[:], c2[:, 2176:2304], reads=["c2"], writes=["SLb"])
        p.cp("dve", UIb[:], c2[:, 2304:2432], reads=["c2"], writes=["UIb"])
        rowmask = c2[:, 2432:2433]
        ecap = c2[:, 2440:2448]
        trash = c2[:, 2448:2449]
        OTt = [p.sb("OTt%d" % i, [128, 8, 128], BF16) for i in range(2)]
        xf5 = [p.sb("x5f%d" % i, [128, D], F32) for i in range(2)]
        y5 = p.sb("y5", [128, D], F32)
        hc = [p.sb("hc%d" % i, [128, D], F32) for i in range(2)]
        hcb = [p.sb("hcb%d" % i, [128, D], BF16) for i in range(2)]
        hcT = p.sb("hcT", [128, 8, 128], F32)
        rt = p.sb("rt", [128, 96], F32)
        ohb = p.sb("ohb", [128, 8], BF16)
        RK = pD

        def ld5(t):
            sl = t % 2
            p.ld("sp", OTt[sl][:], OT_d[:, :, t * 128:(t + 1) * 128].rearrange("h p r -> p h r"),
                 reads=["OT_d"], writes=["OTt%d" % sl])
            p.ld("sp", xf5[sl][:], h_b[t * 128:(t + 1) * 128, :], reads=["h_b_d%d" % t], writes=["x5f%d" % sl])

        ld5(0)
        for t in range(NT):
            sl = t % 2
            if t + 1 < NT:
                ld5(t + 1)
            for hf in range(2):
                bank = PB[hf]
                bkey = "pb%d" % hf
                for kc in range(8):
                    p.mm(bank[:], OTt[sl][:, kc, :], wo[:, kc, hf * 512:(hf + 1) * 512], start=(kc == 0),
                         stop=(kc == 7), reads=["OTt%d" % sl, "wo"], writes=[bkey], signal=(kc == 7))
                p.stt(y5[:, hf * 512:(hf + 1) * 512], xf5[sl][:, hf * 512:(hf + 1) * 512], ALPHA, bank[:],
                      ALU.mult, ALU.add, reads=["x5f%d" % sl, bkey], writes=["y5"])
            hk5 = "hc%d" % sl
            layernorm_tile(p, y5[:], hc[sl][:], gbc[:], bbc[:], sm, "y5", hk5, extra_reads=["gbc", "bbc"])
            p.ld("sp", h_c[t * 128:(t + 1) * 128, :], hc[sl][:], reads=[hk5], writes=["h_c_d%d" % t])
            p.cp("pool", hcb[sl][:], hc[sl][:], reads=[hk5], writes=["hcb%d" % sl])
            for kc in range(8):
                bank = PB[4 + kc // 4]
                p.tr(bank[:, (kc % 4) * 128:(kc % 4 + 1) * 128], hc[sl][:, kc * 128:(kc + 1) * 128], ident_f,
                     reads=[hk5, "cst"], writes=["pb%d" % (4 + kc // 4)], signal=(kc % 4 == 3))
            p.cp("act", hcT[:, 0:4, :].rearrange("p k n -> p (k n)"), PB[4][:], reads=["pb4"], writes=["hcT"])
            p.cp("dve", hcT[:, 4:8, :].rearrange("p k n -> p (k n)"), PB[5][:], reads=["pb5"], writes=["hcT"])
            for kc in range(8):
                p.mm(pC[:, 0:NE], hcT[:, kc, :], wr[:, kc, :], start=(kc == 0), stop=(kc == 7),
                     reads=["hcT", "wr"], writes=["pb2"], signal=(kc == 7))
            p.cp("dve", rt[:, 0:8], pC[:, 0:NE], reads=["pb2"], writes=["rt"])
            p.op("dve", lambda e: e.max(out=rt[:, 8:16], in_=rt[:, 0:8]), reads=["rt"], writes=["rt"])
            p.ts("dve", rt[:, 16:24], rt[:, 0:8], rt[:, 8:9], None, ALU.is_equal, reads=["rt"], writes=["rt"])
            p.ts("dve", rt[:, 24:32], rt[:, 0:8], rt[:, 9:10], None, ALU.is_equal, reads=["rt"], writes=["rt"])
            p.tt("dve", rt[:, 32:40], rt[:, 16:24], rt[:, 24:32], ALU.add, reads=["rt"], writes=["rt"])
            if t % TPS == 0:
                p.ts("dve", rt[:, 32:40], rt[:, 32:40], rowmask, None, ALU.mult, reads=["rt", "c2"], writes=["rt"])
            p.cp("dve", ohb[:], rt[:, 32:40], reads=["rt"], writes=["ohb"])
            p.mm(RK[:, 0:NE], SLb[:], ohb[:], start=(t == 0), stop=False, reads=["SLb", "ohb"], writes=["pb3"])
            p.cp("dve", rt[:, 40:48], RK[:, 0:NE], reads=["pb3"], writes=["rt"])
            p.mm(RK[:, 0:NE], UIb[:], ohb[:], start=False, stop=(t == NT - 1), reads=["UIb", "ohb"],
                 writes=["pb3"])
            p.tt("dve", rt[:, 56:57], rt[:, 8:9], rt[:, 9:10], ALU.subtract, reads=["rt"], writes=["rt"])
            p.act(rt[:, 57:58], rt[:, 56:57], AF.Sigmoid, reads=["rt"], writes=["rt"])
            p.ts("dve", rt[:, 58:59], rt[:, 57:58], -1.0, 1.0, ALU.mult, ALU.add, reads=["rt"], writes=["rt"])
            for k2 in range(2):
                oh = rt[:, 16 + 8 * k2:24 + 8 * k2]
                p.tt("dve", rt[:, 48:56], oh, rt[:, 40:48], ALU.mult, reads=["rt"], writes=["rt"])
                p.op("dve", lambda e: e.reduce_sum(out=rt[:, 60:61], in_=rt[:, 48:56], axis=AX.X),
                     reads=["rt"], writes=["rt"])
                p.tt("dve", rt[:, 48:56], oh, ecap, ALU.mult, reads=["rt", "c2"], writes=["rt"])
                p.op("dve", lambda e: e.reduce_sum(out=rt[:, 61:62], in_=rt[:, 48:56], axis=AX.X),
                     reads=["rt"], writes=["rt"])
                p.ts("dve", rt[:, 62:63], rt[:, 60:61], float(CAP), None, ALU.is_lt, reads=["rt"], writes=["rt"])
                if t % TPS == 0:
                    p.tt("dve", rt[:, 62:63], rt[:, 62:63], rowmask, ALU.mult, reads=["rt", "c2"], writes=["rt"])
                p.tt("dve", rt[:, 63:64], rt[:, 60:61], rt[:, 61:62], ALU.add, reads=["rt"], writes=["rt"])
                p.tt("dve", rt[:, 63:64], rt[:, 63:64], trash, ALU.subtract, reads=["rt", "c2"], writes=["rt"])
                p.tt("dve", rt[:, 63:64], rt[:, 63:64], rt[:, 62:63], ALU.mult, reads=["rt"], writes=["rt"])
                p.tt("dve", rt[:, 63:64], rt[:, 63:64], trash, ALU.add, reads=["rt", "c2"], writes=["rt"])
                p.cp("dve", rpos[:, t, k2:k2 + 1], rt[:, 63:64], reads=["rt"], writes=["rpos"])
                p.tt("dve", rinfo[:, t, k2:k2 + 1], rt[:, 57 + k2:58 + k2], rt[:, 62:63], ALU.mult,
                     reads=["rt"], writes=["rinfo"])
                p.dma("pool", lambda e, t=t, k2=k2, sl=sl: e.indirect_dma_start(
                    out=Xslot[:, :], out_offset=bass.IndirectOffsetOnAxis(ap=rpos[:, t, k2:k2 + 1], axis=0),
                    in_=hcb[sl][:], in_offset=None),
                    reads=["hcb%d" % sl, "rpos", "Xslot"], writes=["Xslot_s"])
        p.phase_end()
        dbg_src = h_c
        n_dbg = NT

    if stop_phase >= 6:
        p.phase_begin()
        NTI = CAP // 128
        XT = p.sb("XT", [128, 8, CAP], BF16)
        Y = p.sb("Y", [128, NTI, D], F32)
        xs = [p.sb("xs%d" % i, [128, D], BF16) for i in range(2)]
        wgc = [p.sb("wgc%d" % i, [128, 8, 512], BF16) for i in range(2)]
        wuc = [p.sb("wuc%d" % i, [128, 8, 512], BF16) for i in range(2)]
        wdc = [p.sb("wdc%d" % i, [128, 4, D], BF16) for i in range(2)]
        HT6 = [p.sb("HT6_%d" % i, [128, 4, 512], BF16) for i in range(2)]
        sg6 = [p.sb("sg6_%d" % i, [128, 512], F32) for i in range(2)]
        NCH = DFE // 512
        chunks = [(e_, cf) for e_ in range(NE) for cf in range(NCH)]

        def ldw(ci):
            e_, cf = chunks[ci]
            sl = ci % 2
            cs = slice(cf * 512, (cf + 1) * 512)
            p.ld("pool", wgc[sl][:], w_ge[e_, :, cs].rearrange("(k p) n -> p k n", p=128), writes=["wgc%d" % sl])
            p.ld("pool", wuc[sl][:], w_ue[e_, :, cs].rearrange("(k p) n -> p k n", p=128), writes=["wuc%d" % sl])
            p.ld("pool", wdc[sl][:], w_de[e_, cs, :].rearrange("(f p) n -> p f n", p=128), writes=["wdc%d" % sl])

        ldw(0)
        tg_groups = [(g0, min(512, CAP - g0)) for g0 in range(0, CAP, 512)]
        gcount = 0
        for ci, (e_, cf) in enumerate(chunks):
            sl = ci % 2
            if cf == 0:
                for i in range(NTI):
                    x_ = xs[i % 2]
                    p.ld("sp", x_[:], Xslot[e_ * CAP + i * 128:e_ * CAP + (i + 1) * 128, :],
                         reads=["Xslot", "Xslot_s"], writes=["xs%d" % (i % 2)])
                    transpose_rows(p, x_, XT, i * 128, "xs%d" % (i % 2), "XT")
            if ci + 1 < len(chunks):
                ldw(ci + 1)
            for (g0, gw) in tg_groups:
                hb6 = HT6[gcount % 2]
                hk6 = "HT6_%d" % (gcount % 2)
                for fb in range(4):
                    gb, ub = PB[fb % 2], PB[2 + fb % 2]
                    gk_, uk_ = "pb%d" % (fb % 2), "pb%d" % (2 + fb % 2)
                    for kc in range(8):
                        p.mm(gb[:, 0:gw], wgc[sl][:, kc, fb * 128:(fb + 1) * 128], XT[:, kc, g0:g0 + gw],
                             start=(kc == 0), stop=(kc == 7), reads=["XT", "wgc%d" % sl], writes=[gk_],
                             signal=(kc == 7))
                    for kc in range(8):
                        p.mm(ub[:, 0:gw], wuc[sl][:, kc, fb * 128:(fb + 1) * 128], XT[:, kc, g0:g0 + gw],
                             start=(kc == 0), stop=(kc == 7), reads=["XT", "wuc%d" % sl], writes=[uk_],
                             signal=(kc == 7))
                    p.act(sg6[fb % 2][:, 0:gw], gb[:, 0:gw], AF.Silu, reads=[gk_], writes=["sg6_%d" % (fb % 2)])
                    p.tt("dve", hb6[:, fb, 0:gw], sg6[fb % 2][:, 0:gw], ub[:, 0:gw], ALU.mult,
                         reads=["sg6_%d" % (fb % 2), uk_], writes=[hk6])
                for j in range(gw // 128):
                    ti = g0 // 128 + j
                    for hf in range(2):
                        bank = PB[4 + hf]
                        bkey = "pb%d" % (4 + hf)
                        for fb in range(4):
                            p.mm(bank[:], hb6[:, fb, j * 128:(j + 1) * 128], wdc[sl][:, fb, hf * 512:(hf + 1) * 512],
                                 start=(fb == 0), stop=(fb == 3), reads=[hk6, "wdc%d" % sl], writes=[bkey],
                                 signal=(fb == 3))
                        ysl = Y[:, ti, hf * 512:(hf + 1) * 512]
                        if cf == 0:
                            if hf == 0:
                                p.cp("act", ysl, bank[:], reads=[bkey], writes=["Y"])
                            else:
                                p.cp("pool" if False else "dve", ysl, bank[:], reads=[bkey], writes=["Y"])
                        else:
                            p.tt("dve", ysl, ysl, bank[:], ALU.add, reads=["Y", bkey], writes=["Y"])
                gcount += 1
            if cf == NCH - 1:
                p.ld("sp", Yslot[e_ * CAP:(e_ + 1) * CAP, :].rearrange("(i p) d -> p i d", p=128), Y[:],
                     reads=["Y"], writes=["Yslot"])
        p.phase_end()

    if stop_phase >= 7:
        p.phase_begin()
        p.ld("sp", gbc[:], lng[3:4, :].partition_broadcast(128), writes=["gbc"])
        p.ld("sp", bbc[:], lnb[3:4, :].partition_broadcast(128), writes=["bbc"])
        Y1 = [p.sb("Y1_%d" % i, [128, D], F32) for i in range(2)]
        Y2 = [p.sb("Y2_%d" % i, [128, D], F32) for i in range(2)]
        xf7 = [p.sb("x7f%d" % i, [128, D], F32) for i in range(2)]
        y7 = p.sb("y7", [128, D], F32)
        ob = [p.sb("ob%d" % i, [128, D], F32) for i in range(2)]

        def ld7(t):
            sl = t % 2
            p.ld("sp", xf7[sl][:], h_c[t * 128:(t + 1) * 128, :], reads=["h_c_d%d" % t], writes=["x7f%d" % sl])
            for k2, Yk in enumerate((Y1, Y2)):
                p.dma("pool", lambda e, t=t, k2=k2, Yk=Yk, sl=sl: e.indirect_dma_start(
                    out=Yk[sl][:], out_offset=None, in_=Yslot[:, :],
                    in_offset=bass.IndirectOffsetOnAxis(ap=rpos[:, t, k2:k2 + 1], axis=0)),
                    reads=["Yslot", "Yzero", "rpos"], writes=["Y%d_%d" % (k2 + 1, sl)])

        ld7(0)
        for t in range(NT):
            sl = t % 2
            if t + 1 < NT:
                ld7(t + 1)
            p.ts("dve", y7[:], Y1[sl][:], rinfo[:, t, 0:1], None, ALU.mult, reads=["Y1_%d" % sl, "rinfo"],
                 writes=["y7"])
            p.stt(y7[:], Y2[sl][:], rinfo[:, t, 1:2], y7[:], ALU.mult, ALU.add, reads=["Y2_%d" % sl, "rinfo", "y7"],
                  writes=["y7"])
            p.stt(y7[:], xf7[sl][:], ALPHA, y7[:], ALU.mult, ALU.add, reads=["x7f%d" % sl, "y7"], writes=["y7"])
            layernorm_tile(p, y7[:], ob[sl][:], gbc[:], bbc[:], sm, "y7", "ob%d" % sl, extra_reads=["gbc", "bbc"])
            p.ld("sp", out_d[t * 128:(t + 1) * 128, :], ob[sl][:], reads=["ob%d" % sl], writes=["out%d" % t])
        p.wait_all("sp", ["out%d" % t for t in range(NT)])
        p.phase_end(final=True)
        return nc

    p.phase_begin()
    dbg = p.sb("dbg", [128, D], F32)
    for t in range(n_dbg):
        p.ld("sp", dbg[:], dbg_src[t * 128:(t + 1) * 128, :], writes=["dbg"])
        p.ld("sp", out_d[t * 128:(t + 1) * 128, :], dbg[:], reads=["dbg"], writes=["out%d" % t])
    p.wait_all("sp", ["out%d" % t for t in range(n_dbg)])
    p.phase_end(final=True)
    return nc


def make_consts():
    c = np.zeros((128, 1024), np.float32)
    c[:, 0:128] = np.eye(128, dtype=np.float32)
    s = np.arange(128)[:, None]
    t = np.arange(128)[None, :]
    c[:, 128:256] = (s <= t).astype(np.float32)
    for h in range(4):
        c[h, 256 + h * 128:256 + (h + 1) * 128] = 1.0
    return c


def make_consts2():
    c = np.zeros((128, 4096), np.float32)
    k = np.arange(128)[:, None]
    q = np.arange(512)[None, :]
    for r in range(4):
        c[:, r * 512:(r + 1) * 512] = ((128 * r + k) < q).astype(np.float32)
    kk = np.arange(128)[None, :]
    c[:, 2048:2176] = (k > kk).astype(np.float32)
    c[:, 2176:2304] = (k < kk).astype(np.float32)
    c[:, 2304:2432] = (k >= kk).astype(np.float32)
    c[:, 2432] = (np.arange(128) >= PAD).astype(np.float32)
    c[:, 2440:2448] = (np.arange(NE) * CAP).astype(np.float32)[None, :]
    c[:, 2448] = (NE * CAP + np.arange(128)).astype(np.float32)
    return c


def make_in_maps(inputs):
    x = np.asarray(inputs["x"], np.float32)
    meta = np.asarray(inputs["meta"], np.float32)
    bg = np.asarray(inputs["b_gate_a"], np.float32)[0]
    shared = {
        "w_in": np.ascontiguousarray(inputs["w_in_a"][0]),
        "bgate": np.ascontiguousarray(np.stack([bg[0:4], bg[4:8]], axis=1)),
        "norm_pk": np.ascontiguousarray(np.asarray(inputs["norm_a"], np.float32)[0].reshape(8, 128).T),
        "w_out": np.ascontiguousarray(inputs["w_out_a"][0]),
        "lng": np.ascontiguousarray(np.asarray(inputs["ln_g"], np.float32).reshape(4, D)),
        "lnb": np.ascontiguousarray(np.asarray(inputs["ln_b"], np.float32).reshape(4, D)),
        "consts": make_consts(),
        "consts2": make_consts2(),
        "w_gd": np.ascontiguousarray(inputs["w_gate_d"][0]),
        "w_ud": np.ascontiguousarray(inputs["w_up_d"][0]),
        "w_dd": np.ascontiguousarray(inputs["w_down_d"][0]),
        "w_kv": np.ascontiguousarray(inputs["w_kv"]),
        "w_q": np.ascontiguousarray(inputs["w_q_b"][0]),
        "w_o": np.ascontiguousarray(inputs["w_o_b"][0]),
        "w_r": np.ascontiguousarray(inputs["w_router"][0]),
        "w_ge": np.ascontiguousarray(inputs["w_gate_e"][0]),
        "w_ue": np.ascontiguousarray(inputs["w_up_e"][0]),
        "w_de": np.ascontiguousarray(inputs["w_down_e"][0]),
    }
    maps = []
    for c in range(NCORES):
        hin = np.zeros((2, RS, D), np.float32)
        for s in range(2):
            hin[s, PAD:128] = meta
            hin[s, 128:] = x[2 * c + s]
        m = dict(shared)
        m["hin"] = hin.reshape(R, D)
        maps.append(m)
    return maps


def kernel(**inputs):
    nc = build()
    maps = make_in_maps(inputs)
    res = run_bass_kernel_spmd(nc, maps, core_ids=list(range(NCORES)))
    out = np.zeros((16, SEQ, D), np.float32)
    for c in range(NCORES):
        o = res.results[c]["out"].reshape(2, RS, D)
        out[2 * c:2 * c + 2] = o[:, 128:, :]
    return out
```

```python
import numpy as np
from contextlib import ExitStack
import concourse.bass as bass
import concourse.mybir as mybir
from concourse.bass_utils import run_bass_kernel_spmd

F32 = mybir.dt.float32
BF16 = mybir.dt.bfloat16
I32 = mybir.dt.int32
U32 = mybir.dt.uint32
AF = mybir.ActivationFunctionType
ALU = mybir.AluOpType
AX = mybir.AxisListType

NDSEM = 8
NCORES = 8
D = 1024
SEQ = 4096
NMETA = 16
TPS = 33
RS = TPS * 128
R = 2 * RS
NT = 2 * TPS
PAD = 112
ALPHA = 4.0 ** 0.25
EPS = 1e-5
DFF = 2816
NE = 8
DFE = 3584
CAP = 2560


class Prog:
    ENGS = ("pe", "act", "dve", "pool", "sp")

    def __init__(self, nc):
        self.nc = nc
        self.ges = ExitStack()
        self.es = self.ges
        self.stream = {e: [] for e in self.ENGS}
        self.cnt = {e: 0 for e in self.ENGS}
        self.waited = {e: {} for e in self.ENGS}
        self.res_w = {}
        self.res_r = {}
        self.dq_n = {q: 0 for q in ("sp", "act", "pool")}
        self.sems = {}
        for e in self.ENGS:
            self.sems[("e", e)] = self.ges.enter_context(nc.semaphore("s_e_" + e))
        for q in ("sp", "act", "pool"):
            for i in range(NDSEM):
                self.sems[("d", q, i)] = self.ges.enter_context(nc.semaphore("s_d_%s_%d" % (q, i)))

    def phase_begin(self):
        self.es = ExitStack()

    def barrier(self):
        for e in self.ENGS:
            waits = {}
            for e2 in self.ENGS:
                if e2 != e and self.cnt[e2] > 0:
                    self._need(e, (("e", e2), self.cnt[e2], e2), waits)
            for q in ("sp", "act", "pool"):
                n = self.dq_n[q]
                for slot in range(NDSEM):
                    k = (n - slot + NDSEM - 1) // NDSEM
                    if k > 0:
                        self._need(e, (("d", q, slot), 16 * k, "dma"), waits)
            self.stream[e].append((None, waits, None, 0))

    def phase_end(self, final=False):
        self.barrier()
        self.emit_block()
        if self.es is not self.ges:
            self.es.close()
        self.es = self.ges
        if final:
            self.ges.close()

    def sb(self, name, shape, dtype):
        self.nalloc = getattr(self, "nalloc", 0) + 1
        return self.es.enter_context(self.nc.sbuf_tensor("%s_%d" % (name, self.nalloc), list(shape), dtype))

    def ps(self, name, shape, dtype):
        return self.es.enter_context(self.nc.psum_tensor(name, list(shape), dtype))

    def dram(self, name, shape, dtype, kind="Internal"):
        return self.nc.dram_tensor(name, list(shape), dtype, kind=kind).ap()

    def _need(self, eng, tok, waits):
        if tok is None:
            return
        semkey, val, src = tok
        if src == "pe" and eng == "pe":
            return
        if self.waited[eng].get(semkey, 0) >= val:
            return
        self.waited[eng][semkey] = val
        waits[semkey] = max(waits.get(semkey, 0), val)

    def _deps(self, eng, reads, writes):
        waits = {}
        for r in reads:
            self._need(eng, self.res_w.get(r), waits)
        for w in writes:
            self._need(eng, self.res_w.get(w), waits)
            for t in self.res_r.get(w, ()):
                self._need(eng, t, waits)
        return waits

    def _record(self, tok, reads, writes):
        for r in reads:
            self.res_r.setdefault(r, []).append(tok)
        for w in writes:
            self.res_w[w] = tok
            self.res_r[w] = []

    def op(self, eng, fn, reads=(), writes=(), signal=True):
        waits = self._deps(eng, reads, writes)
        if signal:
            self.cnt[eng] += 1
            tok = (("e", eng), self.cnt[eng], eng)
        else:
            tok = (("e", eng), self.cnt[eng] + 1, eng)
        self._record(tok, reads, writes)
        self.stream[eng].append((fn, waits, ("e", eng) if signal else None, 1))

    def dma(self, q, fn, reads=(), writes=()):
        waits = self._deps(q, reads, writes)
        i = self.dq_n[q]
        self.dq_n[q] += 1
        slot = i % NDSEM
        semkey = ("d", q, slot)
        prev = 16 * (i // NDSEM)
        if prev > 0 and self.waited[q].get(semkey, 0) < prev:
            self.waited[q][semkey] = prev
            waits[semkey] = max(waits.get(semkey, 0), prev)
        tok = (semkey, prev + 16, "dma")
        self._record(tok, reads, writes)
        self.stream[q].append((fn, waits, semkey, 16))

    def wait_all(self, eng, keys):
        waits = {}
        for k in keys:
            self._need(eng, self.res_w.get(k), waits)
        self.stream[eng].append((None, waits, None, 0))

    def mm(self, out, lhsT, rhs, start=True, stop=True, reads=(), writes=(), signal=True):
        self.op("pe", lambda e: e.matmul(out, lhsT, rhs, start=start, stop=stop),
                reads, writes, signal)

    def tr(self, out, in_, ident, reads=(), writes=(), signal=True):
        self.op("pe", lambda e: e.transpose(out, in_, ident), reads, writes, signal)

    def act(self, out, in_, func, bias=None, scale=None, reads=(), writes=()):
        kw = {}
        if bias is not None:
            kw["bias"] = bias
        if scale is not None:
            kw["scale"] = scale
        self.op("act", lambda e: e.activation(out=out, in_=in_, func=func, **kw), reads, writes)

    def tt(self, eng, out, in0, in1, op, reads=(), writes=()):
        self.op(eng, lambda e: e.tensor_tensor(out=out, in0=in0, in1=in1, op=op), reads, writes)

    def ts(self, eng, out, in0, s1, s2, op0, op1=None, reads=(), writes=()):
        if op1 is None:
            self.op(eng, lambda e: e.tensor_scalar(out=out, in0=in0, scalar1=s1, scalar2=None, op0=op0),
                    reads, writes)
        else:
            self.op(eng, lambda e: e.tensor_scalar(out=out, in0=in0, scalar1=s1, scalar2=s2, op0=op0, op1=op1),
                    reads, writes)

    def stt(self, out, in0, scalar, in1, op0, op1, reads=(), writes=()):
        self.op("dve", lambda e: e.scalar_tensor_tensor(out=out, in0=in0, scalar=scalar, in1=in1,
                                                        op0=op0, op1=op1), reads, writes)

    def cp(self, eng, out, in_, reads=(), writes=()):
        if eng == "act":
            self.op("act", lambda e: e.copy(out=out, in_=in_), reads, writes)
        else:
            self.op(eng, lambda e: e.tensor_copy(out=out, in_=in_), reads, writes)

    def ld(self, q, out, in_, reads=(), writes=()):
        self.dma(q, lambda e: e.dma_start(out=out, in_=in_), reads, writes)

    def emit_block(self):
        nc = self.nc
        with nc.Block() as block:
            handles = {"pe": block.tensor, "act": block.scalar, "dve": block.vector,
                       "pool": block.gpsimd, "sp": block.sync}

            def mk(e):
                def body(engh):
                    for fn, waits, sk, inc in self.stream[e]:
                        for k, v in waits.items():
                            engh.wait_ge(self.sems[k], v)
                        if fn is None:
                            continue
                        ins = fn(engh)
                        if sk is not None:
                            ins.then_inc(self.sems[sk], inc)
                return body

            for e in self.ENGS:
                if self.stream[e]:
                    handles[e](mk(e))
        self.stream = {e: [] for e in self.ENGS}


def load_w_bf(p, name, w_ap, K, N, key):
    t = p.sb(name, [128, K // 128, N], BF16)
    src = w_ap.rearrange("(k p) n -> p k n", p=128)
    c0 = 0
    while c0 < N:
        c1 = min(N, c0 + 2048)
        p.ld("pool", t[:, :, c0:c1], src[:, :, c0:c1], writes=[key])
        c0 = c1
    return t


def layernorm_tile(p, y, out, gbc, bbc, sm, key_y, key_out, extra_reads=()):
    st = sm[:, 0:12].rearrange("p (a b) -> p a b", a=2)
    for hf in range(2):
        p.op("dve", lambda e, hf=hf: e.bn_stats(out=st[:, hf, :], in_=y[:, hf * 512:(hf + 1) * 512]),
             reads=[key_y], writes=["sm_ln"])
    p.op("dve", lambda e: e.bn_aggr(out=sm[:, 12:14], in_=sm[:, 0:12]), reads=["sm_ln"], writes=["sm_ln"])
    p.act(sm[:, 14:15], sm[:, 13:14], AF.Sqrt, bias=sm[:, 20:21], reads=["sm_ln", "sm_eps"], writes=["sm_ln"])
    p.op("dve", lambda e: e.reciprocal(out=sm[:, 15:16], in_=sm[:, 14:15]), reads=["sm_ln"], writes=["sm_ln"])
    p.ts("dve", sm[:, 16:17], sm[:, 12:13], sm[:, 15:16], -1.0, ALU.mult, ALU.mult,
         reads=["sm_ln"], writes=["sm_ln"])
    p.act(out, y, AF.Identity, bias=sm[:, 16:17], scale=sm[:, 15:16], reads=[key_y, "sm_ln"], writes=[key_out])
    p.tt("dve", out, out, gbc, ALU.mult, reads=[key_out] + list(extra_reads), writes=[key_out])
    p.tt("dve", out, out, bbc, ALU.add, reads=[key_out], writes=[key_out])


def build(stop_after=99, stop_phase=7):
    nc = bass.Bass("TRN2", target_bir_lowering=False)
    p = Prog(nc)
    din = lambda n, s: nc.dram_tensor(n, list(s), F32, kind="ExternalInput").ap()
    hin = din("hin", [R, D])
    w_in = din("w_in", [D, 3080])
    bgate = din("bgate", [4, 2])
    norm_pk = din("norm_pk", [128, 8])
    w_out = din("w_out", [D, D])
    lng = din("lng", [4, D])
    lnb = din("lnb", [4, D])
    consts = din("consts", [128, 1024])
    consts2 = din("consts2", [128, 4096])
    w_gd = din("w_gd", [D, DFF])
    w_ud = din("w_ud", [D, DFF])
    w_dd = din("w_dd", [DFF, D])
    w_kv = din("w_kv", [D, 2 * D])
    w_q = din("w_q", [D, D])
    w_o = din("w_o", [D, D])
    w_r = din("w_r", [D, NE])
    w_ge = din("w_ge", [NE, D, DFE])
    w_ue = din("w_ue", [NE, D, DFE])
    w_de = din("w_de", [NE, DFE, D])
    out_d = nc.dram_tensor("out", [R, D], F32, kind="ExternalOutput").ap()
    h_a = p.dram("h_a", [R, D], F32)
    h_b = p.dram("h_b", [R, D], F32)
    h_c = p.dram("h_c", [R, D], F32)
    KT_d = p.dram("KT_d", [8, 128, R], BF16)
    QT_d = p.dram("QT_d", [8, 128, R], BF16)
    V_d = p.dram("V_d", [R, D], BF16)
    OT_d = p.dram("OT_d", [8, 128, R], BF16)
    NSLOT = NE * CAP
    Xslot = p.dram("Xslot", [NSLOT + 128, D], BF16)
    Yslot = p.dram("Yslot", [NSLOT + 128, D], F32)

    cst = p.sb("cst", [128, 1024], F32)
    p.ld("sp", cst[:], consts, writes=["cst"])
    ident_bf = p.sb("ident_bf", [128, 128], BF16)
    p.ld("pool", ident_bf[:], consts[:, 0:128], writes=["ident_bf"])
    ones_bf = p.sb("ones_bf", [128, 8], BF16)
    p.op("dve", lambda e: e.memset(ones_bf[:], 1.0), writes=["ones_bf"])
    ident_f = cst[:, 0:128]
    cmask = cst[:, 128:256]
    sel = cst[0:4, 256:768].rearrange("p (h n) -> p h n", h=4)
    bg = p.sb("bg", [4, 4], F32)
    p.ld("sp", bg[:, 0:2], bgate, writes=["bg"])
    p.ts("dve", bg[:, 2:3], bg[:, 1:2], -1.0, None, ALU.mult, reads=["bg"], writes=["bg"])
    p.op("dve", lambda e: e.memset(bg[:, 3:4], 0.0), reads=["bg"], writes=["bg"])
    ones4 = p.sb("ones4", [4, 128], F32)
    p.op("dve", lambda e: e.memset(ones4[:], 1.0), writes=["ones4"])
    sm = p.sb("sm", [128, 32], F32)
    p.op("dve", lambda e: e.memset(sm[:, 20:21], EPS), writes=["sm_eps"])
    gbc = p.sb("gbc", [128, D], F32)
    bbc = p.sb("bbc", [128, D], F32)

    tpb = p.ps("tpb", [128, 8, 128], BF16)
    PB = [p.ps("pb%d" % i, [128, 512], F32) for i in range(7)]
    pA, pB, pC, pD, pE, pF, pG = PB

    p.phase_begin()
    if True:
        p.ld("sp", gbc[:], lng[0:1, :].partition_broadcast(128), writes=["gbc"])
        p.ld("sp", bbc[:], lnb[0:1, :].partition_broadcast(128), writes=["bbc"])
        win = load_w_bf(p, "win", w_in, D, 3080, "win")
        wout = load_w_bf(p, "wout", w_out, D, D, "wout")
        npk = p.sb("npk", [128, 8], F32)
        p.ld("sp", npk[:], norm_pk, writes=["npk"])
        for kc in range(8):
            p.ts("pool", wout[:, kc, :], wout[:, kc, :], npk[:, kc:kc + 1], None, ALU.mult,
                 reads=["wout", "npk"], writes=["wout"])
        xbf = [p.sb("xbf%d" % i, [128, D], BF16) for i in range(2)]
        xf = [p.sb("xf%d" % i, [128, D], F32) for i in range(2)]
        hT = [p.sb("hT%d" % i, [128, 8, 128], BF16) for i in range(2)]
        G = [p.sb("G%d" % i, [4, 12, 128], F32) for i in range(2)]
        Wbc = p.sb("Wbc", [128, 4, 128], F32)
        tok = p.sb("tok", [128, 12], F32)
        qT = p.sb("qT", [128, 4, 128], BF16)
        kT = p.sb("kT", [128, 4, 128], BF16)
        wk = p.sb("wk", [128, 4, 128], BF16)
        V = p.sb("V", [128, 4, 256], BF16)
        osig = p.sb("osig", [128, D], BF16)
        PT = p.sb("PT", [128, 4, 128], BF16)
        Cf = p.sb("Cf", [128, 4, 256], F32)
        nf = p.sb("nf", [128, 4], F32)
        Cbf = p.sb("Cbf", [128, 4, 256], BF16)
        nbf = p.sb("nbf", [128, 4], BF16)
        nrm = p.sb("nrm", [128, 64], F32)
        hn = p.sb("hn", [128, D], F32)
        outtok = p.sb("outtok", [128, D], BF16)
        outT = p.sb("outT", [128, 8, 128], BF16)
        y = p.sb("y", [128, D], F32)
        ha = [p.sb("ha%d" % i, [128, D], F32) for i in range(2)]
        KSC = 128.0 ** -0.5

        def stage_load(t):
            sl = t % 2
            rows = slice(t * 128, (t + 1) * 128)
            p.ld("pool", xbf[sl][:], hin[rows, :], writes=["xbf%d" % sl])
            p.ld("sp", xf[sl][:], hin[rows, :], writes=["xf%d" % sl])

        stage_load(0)
        for t in range(NT):
            if t >= stop_after:
                break
            sl = t % 2
            c = t % TPS
            first = c == 0
            if t + 1 < NT:
                stage_load(t + 1)
            g = G[sl]
            gp = G[1 - sl]
            gk = "G%d" % sl
            gpk = "G%d" % (1 - sl)
            for kc in range(8):
                p.tr(tpb[:, kc, :], xbf[sl][:, kc * 128:(kc + 1) * 128], ident_bf[:],
                     reads=["xbf%d" % sl, "ident_bf"], writes=["tpb"], signal=(kc == 7))
            p.cp("act", hT[sl][:], tpb[:], reads=["tpb"], writes=["hT%d" % sl])
            hk = "hT%d" % sl
            for gi in range(2):
                for kc in range(8):
                    p.mm(pE[0:4, gi * 128:(gi + 1) * 128], win[:, kc, 3072 + 4 * gi:3076 + 4 * gi],
                         hT[sl][:, kc, :], start=(kc == 0), stop=(kc == 7),
                         reads=[hk, "win"], writes=["pE_g"], signal=(kc == 7))
            p.act(g[:, 0, :], pE[0:4, 0:128], AF.Identity, bias=bg[:, 0:1], reads=["pE_g", "bg"], writes=[gk])
            p.act(g[:, 10, :], pE[0:4, 128:256], AF.Exp, bias=bg[:, 2:3], scale=-1.0,
                  reads=["pE_g", "bg"], writes=[gk])
            p.act(g[:, 11, :], g[:, 10, :], AF.Ln, bias=1.0, reads=[gk], writes=[gk])
            p.ts("dve", g[:, 1, :], g[:, 11, :], -1.0, None, ALU.mult, reads=[gk], writes=[gk])
            if first:
                p.op("dve", lambda e, g=g: e.memset(g[:, 0, 0:PAD], -30000.0), reads=[gk], writes=[gk])
                p.op("dve", lambda e, g=g: e.memset(g[:, 1, 0:PAD], 0.0), reads=[gk], writes=[gk])
                Bi, mi, a0 = bg[:, 3:4], bg[:, 3:4], bg[:, 3:4]
            else:
                Bi, mi, a0 = gp[:, 2, 127:128], gp[:, 3, 127:128], gp[:, 4, 127:128]
            p.op("dve", lambda e, g=g, Bi=Bi: e.tensor_tensor_scan(
                out=g[:, 2, :], data0=ones4[:], data1=g[:, 1, :], initial=Bi, op0=ALU.mult, op1=ALU.add),
                reads=[gk, gpk, "ones4", "bg"], writes=[gk])
            p.op("dve", lambda e, g=g, mi=mi: e.tensor_tensor_scan(
                out=g[:, 3, :], data0=g[:, 1, :], data1=g[:, 0, :], initial=mi, op0=ALU.add, op1=ALU.max),
                reads=[gk, gpk, "bg"], writes=[gk])
            p.tt("dve", g[:, 4, :], g[:, 2, :], g[:, 3, :], ALU.subtract, reads=[gk], writes=[gk])
            p.tt("dve", g[:, 6, :], g[:, 0, :], g[:, 2, :], ALU.subtract, reads=[gk], writes=[gk])
            p.ts("dve", g[:, 10, :], g[:, 4, :], a0, None, ALU.subtract, reads=[gk, gpk, "bg"], writes=[gk])
            p.act(g[:, 5, :], g[:, 10, :], AF.Exp, reads=[gk], writes=[gk])
            p.act(g[:, 7, :], g[:, 6, :], AF.Exp, bias=a0, reads=[gk, gpk, "bg"], writes=[gk])
            p.act(g[:, 8, :], g[:, 6, :], AF.Exp, bias=g[:, 4, 127:128], reads=[gk], writes=[gk])
            p.act(g[:, 9, :], g[:, 3, :], AF.Exp, scale=-1.0, reads=[gk], writes=[gk])
            for h in range(4):
                p.mm(pD[:, h * 128:(h + 1) * 128], sel[:, h, :], g[:, 5, :], reads=[gk, "cst"],
                     writes=["pD"], signal=(h == 3))
            p.cp("act", Wbc[:].rearrange("p h n -> p (h n)"), pD[:], reads=["pD"], writes=["Wbc"])
            for j in range(3):
                p.tr(pE[:, 256 + 4 * j:260 + 4 * j], g[:, 7 + j, :], ident_f[0:4, 0:4],
                     reads=[gk, "cst"], writes=["pE_t"], signal=(j == 2))
            p.cp("dve", tok[:], pE[:, 256:268], reads=["pE_t"], writes=["tok"])
            for h in range(4):
                for kc in range(8):
                    p.mm(pA[:, h * 128:(h + 1) * 128], win[:, kc, h * 128:(h + 1) * 128], hT[sl][:, kc, :],
                         start=(kc == 0), stop=(kc == 7), reads=[hk, "win"], writes=["pA"],
                         signal=(h == 3 and kc == 7))
            p.tt("dve", qT[:].rearrange("p h n -> p (h n)"), pA[:], Wbc[:].rearrange("p h n -> p (h n)"),
                 ALU.mult, reads=["pA", "Wbc"], writes=["qT"])
            for h in range(4):
                for kc in range(8):
                    p.mm(pA[:, h * 128:(h + 1) * 128], win[:, kc, 512 + h * 128:512 + (h + 1) * 128],
                         hT[sl][:, kc, :], start=(kc == 0), stop=(kc == 7), reads=[hk, "win"], writes=["pA"],
                         signal=(h == 3 and kc == 7))
            p.op("act", lambda e: e.mul(out=kT[:].rearrange("p h n -> p (h n)"), in_=pA[:], mul=KSC),
                 reads=["pA"], writes=["kT"])
            banks = [pB, pC]
            bk = ["pB", "pC"]

            def tokmm(bi, col0):
                for kc in range(8):
                    p.mm(banks[bi][:], hT[sl][:, kc, :], win[:, kc, col0:col0 + 512], start=(kc == 0),
                         stop=(kc == 7), reads=[hk, "win"], writes=[bk[bi]], signal=(kc == 7))

            tokmm(0, 512)
            for h in range(4):
                p.ts("dve", wk[:, h, :], pB[:, h * 128:(h + 1) * 128], tok[:, 4 + h:5 + h], KSC,
                     ALU.mult, ALU.mult, reads=["pB", "tok"], writes=["wk"])
            tokmm(1, 1024)
            p.cp("act", V[:, 0:2, :].rearrange("p h n -> p (h n)"), pC[:], reads=["pC"], writes=["V"])
            tokmm(0, 1536)
            p.cp("act", V[:, 2:4, :].rearrange("p h n -> p (h n)"), pB[:], reads=["pB"], writes=["V"])
            tokmm(1, 2048)
            p.act(osig[:, 0:512], pC[:], AF.Sigmoid, reads=["pC"], writes=["osig"])
            tokmm(0, 2560)
            p.act(osig[:, 512:1024], pB[:], AF.Sigmoid, reads=["pB"], writes=["osig"])
            if first:
                p.op("dve", lambda e: e.memset(Cf[:], 0.0), writes=["Cf"])
                p.op("dve", lambda e: e.memset(nf[:], 0.0), writes=["nf"])
                p.op("dve", lambda e: e.memset(Cbf[:], 0.0), writes=["Cbf"])
                p.op("dve", lambda e: e.memset(nbf[:], 0.0), writes=["nbf"])
            for h in range(4):
                p.mm(pA[:, h * 128:(h + 1) * 128], kT[:, h, :], qT[:, h, :], reads=["kT", "qT"], writes=["pA"],
                     signal=(h == 3))
            for h in range(4):
                p.stt(PT[:, h, :], pA[:, h * 128:(h + 1) * 128], tok[:, h:h + 1], cmask, ALU.mult, ALU.mult,
                      reads=["pA", "tok", "cst"], writes=["PT"])
            for h in range(4):
                bank = pF if h < 2 else pG
                bkey = "pF" if h < 2 else "pG"
                reg = bank[:, (h % 2) * 256:(h % 2) * 256 + 256]
                p.mm(reg, PT[:, h, :], V[:, h, :], start=True, stop=False, reads=["PT", "V"], writes=[bkey],
                     signal=False)
                p.mm(reg, qT[:, h, :], Cbf[:, h, :], start=False, stop=True, reads=["qT", "Cbf"], writes=[bkey])
                p.mm(pE[:, 272 + h:273 + h], PT[:, h, :], ones_bf[:, 0:1], start=True, stop=False,
                     reads=["PT", "ones_bf"], writes=["pE_d"], signal=False)
                p.mm(pE[:, 272 + h:273 + h], qT[:, h, :], nbf[:, h:h + 1], start=False, stop=True,
                     reads=["qT", "nbf"], writes=["pE_d"])
            p.act(nrm[:, 0:4], pE[:, 272:276], AF.Abs, reads=["pE_d"], writes=["nrm"])
            p.tt("dve", nrm[:, 0:4], nrm[:, 0:4], tok[:, 8:12], ALU.max, reads=["nrm", "tok"], writes=["nrm"])
            p.op("dve", lambda e: e.reciprocal(out=nrm[:, 0:4], in_=nrm[:, 0:4]), reads=["nrm"], writes=["nrm"])
            for h in range(4):
                bank = pF if h < 2 else pG
                bkey = "pF" if h < 2 else "pG"
                reg = bank[:, (h % 2) * 256:(h % 2) * 256 + 256]
                p.op("dve", lambda e, h=h, reg=reg: e.bn_stats(out=nrm[:, 4 + 6 * h:10 + 6 * h], in_=reg),
                     reads=[bkey, "nrm"], writes=["nrm"])
                p.op("dve", lambda e, h=h: e.bn_aggr(out=nrm[:, 28 + 2 * h:30 + 2 * h],
                                                     in_=nrm[:, 4 + 6 * h:10 + 6 * h]),
                     reads=["nrm"], writes=["nrm"])
            mv = nrm[:, 28:36].rearrange("p (h two) -> p h two", two=2)
            p.tt("dve", nrm[:, 36:40], nrm[:, 0:4], nrm[:, 0:4], ALU.mult, reads=["nrm"], writes=["nrm"])
            p.tt("dve", nrm[:, 36:40], nrm[:, 36:40], mv[:, :, 1], ALU.mult, reads=["nrm"], writes=["nrm"])
            p.act(nrm[:, 36:40], nrm[:, 36:40], AF.Sqrt, bias=sm[:, 20:21], reads=["nrm", "sm_eps"],
                  writes=["nrm"])
            p.op("dve", lambda e: e.reciprocal(out=nrm[:, 36:40], in_=nrm[:, 36:40]), reads=["nrm"],
                 writes=["nrm"])
            p.tt("dve", nrm[:, 40:44], nrm[:, 36:40], nrm[:, 0:4], ALU.mult, reads=["nrm"], writes=["nrm"])
            p.tt("dve", nrm[:, 44:48], nrm[:, 40:44], mv[:, :, 0], ALU.mult, reads=["nrm"], writes=["nrm"])
            p.ts("dve", nrm[:, 44:48], nrm[:, 44:48], -1.0, None, ALU.mult, reads=["nrm"], writes=["nrm"])
            for h in range(4):
                bank = pF if h < 2 else pG
                bkey = "pF" if h < 2 else "pG"
                reg = bank[:, (h % 2) * 256:(h % 2) * 256 + 256]
                p.act(hn[:, h * 256:(h + 1) * 256], reg, AF.Identity, bias=nrm[:, 44 + h:45 + h],
                      scale=nrm[:, 40 + h:41 + h], reads=[bkey, "nrm"], writes=["hn"])
            p.tt("dve", outtok[:], hn[:], osig[:], ALU.mult, reads=["hn", "osig"], writes=["outtok"])
            for h in range(4):
                bank = pF if h < 2 else pG
                bkey = "pF" if h < 2 else "pG"
                reg = bank[:, (h % 2) * 256:(h % 2) * 256 + 256]
                p.mm(reg, wk[:, h, :], V[:, h, :], reads=["wk", "V"], writes=[bkey])
                p.mm(pE[:, 280 + h:281 + h], wk[:, h, :], ones_bf[:, 0:1], reads=["wk", "ones_bf"],
                     writes=["pE_n"])
                dec = Wbc[:, h, 127:128]
                p.stt(Cf[:, h, :], Cf[:, h, :], dec, reg, ALU.mult, ALU.add, reads=["Cf", "Wbc", bkey],
                      writes=["Cf"])
                p.stt(nf[:, h:h + 1], nf[:, h:h + 1], dec, pE[:, 280 + h:281 + h], ALU.mult, ALU.add,
                      reads=["nf", "Wbc", "pE_n"], writes=["nf"])
            p.cp("act", Cbf[:].rearrange("p h n -> p (h n)"), Cf[:].rearrange("p h n -> p (h n)"),
                 reads=["Cf"], writes=["Cbf"])
            p.cp("act", nbf[:], nf[:], reads=["nf"], writes=["nbf"])
            for kc in range(8):
                p.tr(tpb[:, kc, :], outtok[:, kc * 128:(kc + 1) * 128], ident_bf[:],
                     reads=["outtok", "ident_bf"], writes=["tpb"], signal=(kc == 7))
            p.cp("act", outT[:], tpb[:], reads=["tpb"], writes=["outT"])
            for hf in range(2):
                bi = 1 - hf
                for kc in range(8):
                    p.mm(banks[bi][:], outT[:, kc, :], wout[:, kc, hf * 512:(hf + 1) * 512], start=(kc == 0),
                         stop=(kc == 7), reads=["outT", "wout"], writes=[bk[bi]], signal=(kc == 7))
                p.stt(y[:, hf * 512:(hf + 1) * 512], xf[sl][:, hf * 512:(hf + 1) * 512], ALPHA, banks[bi][:],
                      ALU.mult, ALU.add, reads=["xf%d" % sl, bk[bi]], writes=["y"])
            layernorm_tile(p, y[:], ha[sl][:], gbc[:], bbc[:], sm, "y", "ha%d" % sl, extra_reads=["gbc", "bbc"])
            p.ld("sp", h_a[t * 128:(t + 1) * 128, :], ha[sl][:], reads=["ha%d" % sl], writes=["h_a_d%d" % t])

    p.phase_end()
    dbg_src = h_a
    n_dbg = min(NT, stop_after)

    def load_c2(p):
        c2 = p.sb("c2", [128, 4096], F32)
        p.ld("sp", c2[:], consts2, writes=["c2"])
        return c2

    def transpose_rows(p, src_bf, dst, col0, rk, wk_):
        for kc in range(8):
            p.tr(tpb[:, kc, :], src_bf[:, kc * 128:(kc + 1) * 128], ident_bf[:],
                 reads=[rk, "ident_bf"], writes=["tpb"], signal=(kc == 7))
        p.cp("act", dst[:, :, col0:col0 + 128], tpb[:], reads=["tpb"], writes=[wk_])

    if stop_phase >= 2:
        p.phase_begin()
        p.ld("sp", gbc[:], lng[1:2, :].partition_broadcast(128), writes=["gbc"])
        p.ld("sp", bbc[:], lnb[1:2, :].partition_broadcast(128), writes=["bbc"])
        wg = load_w_bf(p, "wg", w_gd, D, DFF, "wg")
        wu = load_w_bf(p, "wu", w_ud, D, DFF, "wu")
        wd = load_w_bf(p, "wd", w_dd, DFF, D, "wd")
        NF = DFF // 128
        xbf2 = [p.sb("x2bf%d" % i, [128, 2, D], BF16) for i in range(2)]
        xf2 = [p.sb("x2f%d" % i, [128, D], F32) for i in range(2)]
        hT2 = p.sb("hT2", [128, 8, 256], BF16)
        HT = p.sb("HT", [128, NF, 256], BF16)
        sg = [p.sb("sg%d" % i, [128, 256], F32) for i in range(2)]
        y2 = p.sb("y2", [128, D], F32)
        hb = [p.sb("hb%d" % i, [128, D], F32) for i in range(2)]
        NG = R // 256

        def ld2(g):
            sl = g % 2
            p.ld("pool", xbf2[sl][:], h_a[g * 256:(g + 1) * 256, :].rearrange("(j p) d -> p j d", p=128),
                 reads=["h_a_d%d" % (2 * g), "h_a_d%d" % (2 * g + 1)], writes=["x2bf%d" % sl])

        ld2(0)
        for g in range(NG):
            sl = g % 2
            if g + 1 < NG:
                ld2(g + 1)
            for j in range(2):
                transpose_rows(p, xbf2[sl][:, j, :], hT2, j * 128, "x2bf%d" % sl, "hT2")
            for f in range(NF):
                gb, ub = PB[f % 2], PB[2 + f % 2]
                gk_, uk_ = "pb%d" % (f % 2), "pb%d" % (2 + f % 2)
                for kc in range(8):
                    p.mm(gb[:, 0:256], wg[:, kc, f * 128:(f + 1) * 128], hT2[:, kc, :], start=(kc == 0),
                         stop=(kc == 7), reads=["hT2", "wg"], writes=[gk_], signal=(kc == 7))
                for kc in range(8):
                    p.mm(ub[:, 0:256], wu[:, kc, f * 128:(f + 1) * 128], hT2[:, kc, :], start=(kc == 0),
                         stop=(kc == 7), reads=["hT2", "wu"], writes=[uk_], signal=(kc == 7))
                p.act(sg[f % 2][:], gb[:, 0:256], AF.Silu, reads=[gk_], writes=["sg%d" % (f % 2)])
                p.tt("dve", HT[:, f, :], sg[f % 2][:], ub[:, 0:256], ALU.mult, reads=["sg%d" % (f % 2), uk_],
                     writes=["HT"])
            for j in range(2):
                t = 2 * g + j
                s2 = t % 2
                p.ld("sp", xf2[s2][:], h_a[t * 128:(t + 1) * 128, :], reads=["h_a_d%d" % t],
                     writes=["x2f%d" % s2])
                for hf in range(2):
                    bank = PB[4 + hf]
                    bkey = "pb%d" % (4 + hf)
                    for f in range(NF):
                        p.mm(bank[:], HT[:, f, j * 128:(j + 1) * 128], wd[:, f, hf * 512:(hf + 1) * 512],
                             start=(f == 0), stop=(f == NF - 1), reads=["HT", "wd"], writes=[bkey],
                             signal=(f == NF - 1))
                    p.stt(y2[:, hf * 512:(hf + 1) * 512], xf2[s2][:, hf * 512:(hf + 1) * 512], ALPHA, bank[:],
                          ALU.mult, ALU.add, reads=["x2f%d" % s2, bkey], writes=["y2"])
                layernorm_tile(p, y2[:], hb[s2][:], gbc[:], bbc[:], sm, "y2", "hb%d" % s2,
                               extra_reads=["gbc", "bbc"])
                p.ld("sp", h_b[t * 128:(t + 1) * 128, :], hb[s2][:], reads=["hb%d" % s2], writes=["h_b_d%d" % t])
        p.phase_end()
        dbg_src = h_b
        n_dbg = NT

    if stop_phase >= 3:
        p.phase_begin()
        wkv = load_w_bf(p, "wkv", w_kv, D, 2 * D, "wkv")
        wq = load_w_bf(p, "wq", w_q, D, D, "wq")
        zt = p.sb("zt", [128, 8192], BF16)
        p.op("pool", lambda e: e.memset(zt[:], 0.0), writes=["zt"])
        zrows = 128 * 8
        for i in range(NSLOT // zrows):
            p.ld("sp", Xslot[i * zrows:(i + 1) * zrows, :].rearrange("(p j) d -> p (j d)", p=128), zt[:],
                 reads=["zt"], writes=["Xslot"])
        zf = p.sb("zf", [128, D], F32)
        p.op("pool", lambda e: e.memset(zf[:], 0.0), writes=["zf"])
        p.ld("sp", Yslot[NSLOT:NSLOT + 128, :], zf[:], reads=["zf"], writes=["Yzero"])
        xbf3 = [p.sb("x3bf%d" % i, [128, 4, D], BF16) for i in range(2)]
        hT3 = p.sb("hT3", [128, 8, 512], BF16)
        kq = [p.sb("kq%d" % i, [128, 512], BF16) for i in range(2)]
        vs = [p.sb("vs%d" % i, [128, D], BF16) for i in range(2)]
        QSC = 128.0 ** -0.5
        groups = [(g * 512, 512) for g in range(R // 512)]
        if R % 512:
            groups.append((R - R % 512, R % 512))

        def ld3(gi):
            r0, w = groups[gi]
            sl = gi % 2
            nj = w // 128
            p.ld("pool", xbf3[sl][:, 0:nj, :], h_b[r0:r0 + w, :].rearrange("(j p) d -> p j d", p=128),
                 reads=["h_b_d%d" % (r0 // 128 + j) for j in range(nj)], writes=["x3bf%d" % sl])

        ld3(0)
        cnt3 = 0
        for gi, (r0, w) in enumerate(groups):
            sl = gi % 2
            nj = w // 128
            if gi + 1 < len(groups):
                ld3(gi + 1)
            for j in range(nj):
                transpose_rows(p, xbf3[sl][:, j, :], hT3, j * 128, "x3bf%d" % sl, "hT3")
            for which in range(2):
                for h in range(8):
                    bank = PB[cnt3 % 4]
                    bkey = "pb%d" % (cnt3 % 4)
                    wsrc = wkv if which == 0 else wq
                    for kc in range(8):
                        p.mm(bank[:, 0:w], wsrc[:, kc, h * 128:(h + 1) * 128], hT3[:, kc, 0:w], start=(kc == 0),
                             stop=(kc == 7), reads=["hT3", "wkv", "wq"], writes=[bkey], signal=(kc == 7))
                    ks = kq[cnt3 % 2]
                    kk = "kq%d" % (cnt3 % 2)
                    if which == 0:
                        p.cp("act", ks[:, 0:w], bank[:, 0:w], reads=[bkey], writes=[kk])
                        p.ld("sp", KT_d[h, :, r0:r0 + w], ks[:, 0:w], reads=[kk], writes=["KT_d"])
                    else:
                        p.op("act", lambda e, ks=ks, bank=bank, w=w: e.mul(out=ks[:, 0:w], in_=bank[:, 0:w],
                                                                             mul=QSC), reads=[bkey], writes=[kk])
                        p.ld("sp", QT_d[h, :, r0:r0 + w], ks[:, 0:w], reads=[kk], writes=["QT_d"])
                    cnt3 += 1
            for j in range(nj):
                t = r0 // 128 + j
                v_ = vs[t % 2]
                vk = "vs%d" % (t % 2)
                for hf in range(2):
                    bank = PB[4 + hf]
                    bkey = "pb%d" % (4 + hf)
                    for kc in range(8):
                        p.mm(bank[:], hT3[:, kc, j * 128:(j + 1) * 128], wkv[:, kc, D + hf * 512:D + (hf + 1) * 512],
                             start=(kc == 0), stop=(kc == 7), reads=["hT3", "wkv"], writes=[bkey],
                             signal=(kc == 7))
                    if hf == 0:
                        p.cp("dve", v_[:, 0:512], bank[:], reads=[bkey], writes=[vk])
                    else:
                        p.cp("act", v_[:, 512:1024], bank[:], reads=[bkey], writes=[vk])
                p.ld("sp", V_d[t * 128:(t + 1) * 128, :], v_[:], reads=[vk], writes=["V_d"])
        p.phase_end()

    if stop_phase >= 4:
        p.phase_begin()
        c2 = load_c2(p)
        Ub = p.sb("Ub", [128, 128], BF16)
        Lb = p.sb("Lb", [128, 128], BF16)
        p.cp("dve", Ub[:], c2[:, 2048:2176], reads=["c2"], writes=["Ub"])
        p.cp("dve", Lb[:], cst[:, 128:256], reads=["cst"], writes=["Lb"])
        rowmask = c2[:, 2432:2433]
        mbf = p.sb("mbf", [128, 2048], BF16)
        p.cp("dve", mbf[:], c2[:, 0:2048], reads=["c2"], writes=["mbf"])
        negm = p.sb("negm", [128, 2048], F32)
        p.ts("dve", negm[:], c2[:, 0:2048], 30000.0, -30000.0, ALU.mult, ALU.add, reads=["c2"], writes=["negm"])
        negrow = p.sb("negrow", [128, 2], F32)
        p.ts("dve", negrow[:, 0:1], rowmask, 30000.0, -30000.0, ALU.mult, ALU.add, reads=["c2"], writes=["negrow"])
        p.op("dve", lambda e: e.memset(negrow[:, 1:2], 0.0), reads=["negrow"], writes=["negrow"])
        KTs = [p.sb("KTs%d" % i, [128, RS], BF16) for i in range(4)]
        QTs = [p.sb("QTs%d" % i, [128, RS], BF16) for i in range(4)]
        Vbs = [p.sb("Vbs%d" % i, [128, TPS, 128], BF16) for i in range(4)]
        Eb = [p.sb("Eb%d" % i, [128, 512], F32) for i in range(2)]
        SPB = [p.sb("SPB%d" % i, [128, 512], BF16) for i in range(4)]
        Tb = [p.sb("Tb%d" % i, [128, 512], F32) for i in range(4)]
        AB = [p.sb("AB%d" % i, [128, 512], BF16) for i in range(4)]
        OTs = [p.sb("OTs%d" % i, [128, 512], BF16) for i in range(2)]

        def ld4(h):
            for s_ in range(2):
                sl = (h % 2) * 2 + s_
                rr = slice(s_ * RS, (s_ + 1) * RS)
                p.ld("sp", KTs[sl][:], KT_d[h, :, rr], writes=["KTs%d" % sl])
                p.ld("sp", QTs[sl][:], QT_d[h, :, rr], writes=["QTs%d" % sl])
                p.ld("sp", Vbs[sl][:], V_d[rr, h * 128:(h + 1) * 128].rearrange("(j p) d -> p j d", p=128),
                     writes=["Vbs%d" % sl])

        steps = []
        first_x = {}
        for h in range(8):
            for qi in range(9):
                jmax = min(4 * qi + 3, TPS - 1)
                for j in range(jmax, -1, -1):
                    for s_ in range(2):
                        first_x.setdefault(h, len(steps))
                        steps.append(dict(s=s_, h=h, qi=qi, j=j, jmax=jmax, x=len(steps),
                                          sl=(h % 2) * 2 + s_))
        NS = len(steps)
        Zb = [PB[0], PB[1], PB[6]]
        zkeys = ["pb0", "pb1", "pb6"]
        NDUMMY = 2

        def geo(st):
            q0 = st["qi"] * 512
            W = min(512, RS - q0)
            return q0, W, st["j"] - 4 * st["qi"]

        def stage1(st):
            x, sl, j = st["x"], st["sl"], st["j"]
            q0, W, r = geo(st)
            if x == first_x[st["h"]] + 6 and st["h"] + 1 < 8:
                ld4(st["h"] + 1)
            Z, zk = Zb[x % 3], zkeys[x % 3]
            b2, b3 = x % 2, x % 4
            p.mm(Z[:, 0:W], KTs[sl][:, j * 128:(j + 1) * 128], QTs[sl][:, q0:q0 + W],
                 reads=["KTs%d" % sl, "QTs%d" % sl], writes=[zk])

        def stage1e(st):
            x, sl, j = st["x"], st["sl"], st["j"]
            q0, W, r = geo(st)
            Z, zk = Zb[x % 3], zkeys[x % 3]
            b2, b3 = x % 2, x % 4
            p.act(Eb[b2][:, 0:W], Z[:, 0:W], AF.Exp, reads=[zk], writes=["Eb%d" % b2])
            p.act(SPB[b3][:, 0:W], Eb[b2][:, 0:W], AF.Ln, bias=1.0, reads=["Eb%d" % b2], writes=["SPB%d" % b3])

        def stage1b(st):
            x, j = st["x"], st["j"]
            q0, W, r = geo(st)
            Z, zk = Zb[x % 3], zkeys[x % 3]
            b2, b3 = x % 2, x % 4
            if r >= 0:
                p.tt("dve", SPB[b3][:, 0:W], SPB[b3][:, 0:W], mbf[:, r * 512:r * 512 + W], ALU.mult,
                     reads=["SPB%d" % b3, "mbf"], writes=["SPB%d" % b3])
            if j == 0:
                p.ts("dve", SPB[b3][:, 0:W], SPB[b3][:, 0:W], rowmask, None, ALU.mult,
                     reads=["SPB%d" % b3, "c2"], writes=["SPB%d" % b3])
            p.tt("dve", Tb[b3][:, 0:W], Z[:, 0:W], SPB[b3][:, 0:W], ALU.subtract, reads=[zk, "SPB%d" % b3],
                 writes=["Tb%d" % b3])
            if r >= 0:
                p.tt("pool", Tb[b3][:, 0:W], Tb[b3][:, 0:W], negm[:, r * 512:r * 512 + W], ALU.add,
                     reads=["Tb%d" % b3, "negm"], writes=["Tb%d" % b3])

        def stage2a(st):
            x, j = st["x"], st["j"]
            q0, W, r = geo(st)
            b3 = x % 4
            A, ak = PB[2 + st["s"]], "pb%d" % (2 + st["s"])
            p.mm(A[:, 0:W], Ub[:], SPB[b3][:, 0:W], start=(j == st["jmax"]), stop=False,
                 reads=["Ub", "SPB%d" % b3], writes=[ak])
            p.tt("dve", Tb[b3][:, 0:W], Tb[b3][:, 0:W], A[:, 0:W], ALU.subtract, reads=["Tb%d" % b3, ak],
                 writes=["Tb%d" % b3])

        def stage2b(st):
            x, j = st["x"], st["j"]
            q0, W, r = geo(st)
            b3 = x % 4
            A, ak = PB[2 + st["s"]], "pb%d" % (2 + st["s"])
            p.mm(A[:, 0:W], Lb[:], SPB[b3][:, 0:W], start=False, stop=(j == 0),
                 reads=["Lb", "SPB%d" % b3], writes=[ak])

        def stage2c(st):
            x, j = st["x"], st["j"]
            q0, W, r = geo(st)
            b3 = x % 4
            p.act(AB[b3][:, 0:W], Tb[b3][:, 0:W], AF.Exp, bias=(negrow[:, 0:1] if j == 0 else negrow[:, 1:2]),
                  reads=["Tb%d" % b3, "negrow"], writes=["AB%d" % b3])

        def stage3(st):
            x, sl, j = st["x"], st["sl"], st["j"]
            q0, W, r = geo(st)
            b3 = x % 4
            O, ok_ = PB[4 + st["s"]], "pb%d" % (4 + st["s"])
            p.mm(O[:, 0:W], Vbs[sl][:, j, :], AB[b3][:, 0:W], start=(j == st["jmax"]), stop=(j == 0),
                 reads=["Vbs%d" % sl, "AB%d" % b3], writes=[ok_])
            if j == 0:
                ot, otk = OTs[st["s"]], "OTs%d" % st["s"]
                p.cp("act", ot[:, 0:W], O[:, 0:W], reads=[ok_], writes=[otk])
                p.ld("sp", OT_d[st["h"], :, st["s"] * RS + q0:st["s"] * RS + q0 + W], ot[:, 0:W], reads=[otk],
                     writes=["OT_d"])

        ld4(0)
        for n in range(NS + 3):
            if 0 <= n - 2 < NS:
                stage2b(steps[n - 2])
            if 0 <= n - 1 < NS:
                stage2a(steps[n - 1])
            if n == 0:
                stage1(steps[0])
            if n + 1 < NS:
                stage1(steps[n + 1])
            if n < NS:
                stage1e(steps[n])
            for _ in range(NDUMMY):
                p.mm(tpb[:].rearrange("p a b -> p (a b)").bitcast(F32), Ub[:], mbf[:, 0:512], reads=["Ub", "mbf"],
                     writes=["tpb"], signal=False)
            if 0 <= n - 3 < NS:
                stage3(steps[n - 3])
            if 0 <= n - 2 < NS:
                stage2c(steps[n - 2])
            if n < NS:
                stage1b(steps[n])
        p.phase_end()

    rinfo = p.sb("rinfo", [128, NT, 4], F32)
    rpos = p.sb("rpos", [128, NT, 2], I32)
    if stop_phase >= 5:
        p.phase_begin()
        c2 = load_c2(p)
        p.ld("sp", gbc[:], lng[2:3, :].partition_broadcast(128), writes=["gbc"])
        p.ld("sp", bbc[:], lnb[2:3, :].partition_broadcast(128), writes=["bbc"])
        wo = load_w_bf(p, "wo", w_o, D, D, "wo")
        wr = p.sb("wr", [128, 8, NE], F32)
        p.ld("sp", wr[:], w_r.rearrange("(k p) e -> p k e", p=128), writes=["wr"])
        SLb = p.sb("SLb", [128, 128], BF16)
        UIb = p.sb("UIb", [128, 128], BF16)
        p.cp("dve", SLb[:], c2[:, 2176:2304], reads=["c2"], writes=["SLb"])
        p.cp("dve", UIb[:], c2[:, 2304:2432], reads=["c2"], writes=["UIb"])
        rowmask = c2[:, 2432:2433]
        ecap = c2[:, 2440:2448]
        trash = c2[:, 2448:2449]
        OTt = [p.sb("OTt%d" % i, [128, 8, 128], BF16) for i in range(2)]
        xf5 = [p.sb("x5f%d" % i, [128, D], F32) for i in range(2)]
        y5 = p.sb("y5", [128, D], F32)
        hc = [p.sb("hc%d" % i, [128, D], F32) for i in range(2)]
        hcb = [p.sb("hcb%d" % i, [128, D], BF16) for i in range(2)]
        hcT = p.sb("hcT", [128, 8, 128], F32)
        rt = p.sb("rt", [128, 96], F32)
        ohb = p.sb("ohb", [128, 8], BF16)
        RK = pD

        def ld5(t):
            sl = t % 2
            p.ld("sp", OTt[sl][:], OT_d[:, :, t * 128:(t + 1) * 128].rearrange("h p r -> p h r"),
                 reads=["OT_d"], writes=["OTt%d" % sl])
            p.ld("sp", xf5[sl][:], h_b[t * 128:(t + 1) * 128, :], reads=["h_b_d%d" % t], writes=["x5f%d" % sl])

        ld5(0)
        for t in range(NT):
            sl = t % 2
            if t + 1 < NT:
                ld5(t + 1)
            for hf in range(2):
                bank = PB[hf]
                bkey = "pb%d" % hf
                for kc in range(8):
                    p.mm(bank[:], OTt[sl][:, kc, :], wo[:, kc, hf * 512:(hf + 1) * 512], start=(kc == 0),
                         stop=(kc == 7), reads=["OTt%d" % sl, "wo"], writes=[bkey], signal=(kc == 7))
                p.stt(y5[:, hf * 512:(hf + 1) * 512], xf5[sl][:, hf * 512:(hf + 1) * 512], ALPHA, bank[:],
                      ALU.mult, ALU.add, reads=["x5f%d" % sl, bkey], writes=["y5"])
            hk5 = "hc%d" % sl
            layernorm_tile(p, y5[:], hc[sl][:], gbc[:], bbc[:], sm, "y5", hk5, extra_reads=["gbc", "bbc"])
            p.ld("sp", h_c[t * 128:(t + 1) * 128, :], hc[sl][:], reads=[hk5], writes=["h_c_d%d" % t])
            p.cp("pool", hcb[sl][:], hc[sl][:], reads=[hk5], writes=["hcb%d" % sl])
            for kc in range(8):
                bank = PB[4 + kc // 4]
                p.tr(bank[:, (kc % 4) * 128:(kc % 4 + 1) * 128], hc[sl][:, kc * 128:(kc + 1) * 128], ident_f,
                     reads=[hk5, "cst"], writes=["pb%d" % (4 + kc // 4)], signal=(kc % 4 == 3))
            p.cp("act", hcT[:, 0:4, :].rearrange("p k n -> p (k n)"), PB[4][:], reads=["pb4"], writes=["hcT"])
            p.cp("dve", hcT[:, 4:8, :].rearrange("p k n -> p (k n)"), PB[5][:], reads=["pb5"], writes=["hcT"])
            for kc in range(8):
                p.mm(pC[:, 0:NE], hcT[:, kc, :], wr[:, kc, :], start=(kc == 0), stop=(kc == 7),
                     reads=["hcT", "wr"], writes=["pb2"], signal=(kc == 7))
            p.cp("dve", rt[:, 0:8], pC[:, 0:NE], reads=["pb2"], writes=["rt"])
            p.op("dve", lambda e: e.max(out=rt[:, 8:16], in_=rt[:, 0:8]), reads=["rt"], writes=["rt"])
            p.ts("dve", rt[:, 16:24], rt[:, 0:8], rt[:, 8:9], None, ALU.is_equal, reads=["rt"], writes=["rt"])
            p.ts("dve", rt[:, 24:32], rt[:, 0:8], rt[:, 9:10], None, ALU.is_equal, reads=["rt"], writes=["rt"])
            p.tt("dve", rt[:, 32:40], rt[:, 16:24], rt[:, 24:32], ALU.add, reads=["rt"], writes=["rt"])
            if t % TPS == 0:
                p.ts("dve", rt[:, 32:40], rt[:, 32:40], rowmask, None, ALU.mult, reads=["rt", "c2"], writes=["rt"])
            p.cp("dve", ohb[:], rt[:, 32:40], reads=["rt"], writes=["ohb"])
            p.mm(RK[:, 0:NE], SLb[:], ohb[:], start=(t == 0), stop=False, reads=["SLb", "ohb"], writes=["pb3"])
            p.cp("dve", rt[:, 40:48], RK[:, 0:NE], reads=["pb3"], writes=["rt"])
            p.mm(RK[:, 0:NE], UIb[:], ohb[:], start=False, stop=(t == NT - 1), reads=["UIb", "ohb"],
                 writes=["pb3"])
            p.tt("dve", rt[:, 56:57], rt[:, 8:9], rt[:, 9:10], ALU.subtract, reads=["rt"], writes=["rt"])
            p.act(rt[:, 57:58], rt[:, 56:57], AF.Sigmoid, reads=["rt"], writes=["rt"])
            p.ts("dve", rt[:, 58:59], rt[:, 57:58], -1.0, 1.0, ALU.mult, ALU.add, reads=["rt"], writes=["rt"])
            for k2 in range(2):
                oh = rt[:, 16 + 8 * k2:24 + 8 * k2]
                p.tt("dve", rt[:, 48:56], oh, rt[:, 40:48], ALU.mult, reads=["rt"], writes=["rt"])
                p.op("dve", lambda e: e.reduce_sum(out=rt[:, 60:61], in_=rt[:, 48:56], axis=AX.X),
                     reads=["rt"], writes=["rt"])
                p.tt("dve", rt[:, 48:56], oh, ecap, ALU.mult, reads=["rt", "c2"], writes=["rt"])
                p.op("dve", lambda e: e.reduce_sum(out=rt[:, 61:62], in_=rt[:, 48:56], axis=AX.X),
                     reads=["rt"], writes=["rt"])
                p.ts("dve", rt[:, 62:63], rt[:, 60:61], float(CAP), None, ALU.is_lt, reads=["rt"], writes=["rt"])
                if t % TPS == 0:
                    p.tt("dve", rt[:, 62:63], rt[:, 62:63], rowmask, ALU.mult, reads=["rt", "c2"], writes=["rt"])
                p.tt("dve", rt[:, 63:64], rt[:, 60:61], rt[:, 61:62], ALU.add, reads=["rt"], writes=["rt"])
                p.tt("dve", rt[:, 63:64], rt[:, 63:64], trash, ALU.subtract, reads=["rt", "c2"], writes=["rt"])
                p.tt("dve", rt[:, 63:64], rt[:, 63:64], rt[:, 62:63], ALU.mult, reads=["rt"], writes=["rt"])
                p.tt("dve", rt[:, 63:64], rt[:, 63:64], trash, ALU.add, reads=["rt", "c2"], writes=["rt"])
                p.cp("dve", rpos[:, t, k2:k2 + 1], rt[:, 63:64], reads=["rt"], writes=["rpos"])
                p.tt("dve", rinfo[:, t, k2:k2 + 1], rt[:, 57 + k2:58 + k2], rt[:, 62:63], ALU.mult,
                     reads=["rt"], writes=["rinfo"])
                p.dma("pool", lambda e, t=t, k2=k2, sl=sl: e.indirect_dma_start(
                    out=Xslot[:, :], out_offset=bass.IndirectOffsetOnAxis(ap=rpos[:, t, k2:k2 + 1], axis=0),
                    in_=hcb[sl][:], in_offset=None),
                    reads=["hcb%d" % sl, "rpos", "Xslot"], writes=["Xslot_s"])
        p.phase_end()
        dbg_src = h_c
        n_dbg = NT

    if stop_phase >= 6:
        p.phase_begin()
        NTI = CAP // 128
        XT = p.sb("XT", [128, 8, CAP], BF16)
        Y = p.sb("Y", [128, NTI, D], F32)
        xs = [p.sb("xs%d" % i, [128, D], BF16) for i in range(2)]
        wgc = [p.sb("wgc%d" % i, [128, 8, 512], BF16) for i in range(2)]
        wuc = [p.sb("wuc%d" % i, [128, 8, 512], BF16) for i in range(2)]
        wdc = [p.sb("wdc%d" % i, [128, 4, D], BF16) for i in range(2)]
        HT6 = [p.sb("HT6_%d" % i, [128, 4, 512], BF16) for i in range(2)]
        sg6 = [p.sb("sg6_%d" % i, [128, 512], F32) for i in range(2)]
        NCH = DFE // 512
        chunks = [(e_, cf) for e_ in range(NE) for cf in range(NCH)]

        def ldw(ci):
            e_, cf = chunks[ci]
            sl = ci % 2
            cs = slice(cf * 512, (cf + 1) * 512)
            p.ld("pool", wgc[sl][:], w_ge[e_, :, cs].rearrange("(k p) n -> p k n", p=128), writes=["wgc%d" % sl])
            p.ld("pool", wuc[sl][:], w_ue[e_, :, cs].rearrange("(k p) n -> p k n", p=128), writes=["wuc%d" % sl])
            p.ld("pool", wdc[sl][:], w_de[e_, cs, :].rearrange("(f p) n -> p f n", p=128), writes=["wdc%d" % sl])

        ldw(0)
        tg_groups = [(g0, min(512, CAP - g0)) for g0 in range(0, CAP, 512)]
        gcount = 0
        for ci, (e_, cf) in enumerate(chunks):
            sl = ci % 2
            if cf == 0:
                for i in range(NTI):
                    x_ = xs[i % 2]
                    p.ld("sp", x_[:], Xslot[e_ * CAP + i * 128:e_ * CAP + (i + 1) * 128, :],
                         reads=["Xslot", "Xslot_s"], writes=["xs%d" % (i % 2)])
                    transpose_rows(p, x_, XT, i * 128, "xs%d" % (i % 2), "XT")
            if ci + 1 < len(chunks):
                ldw(ci + 1)
            for (g0, gw) in tg_groups:
                hb6 = HT6[gcount % 2]
                hk6 = "HT6_%d" % (gcount % 2)
                for fb in range(4):
                    gb, ub = PB[fb % 2], PB[2 + fb % 2]
                    gk_, uk_ = "pb%d" % (fb % 2), "pb%d" % (2 + fb % 2)
                    for kc in range(8):
                        p.mm(gb[:, 0:gw], wgc[sl][:, kc, fb * 128:(fb + 1) * 128], XT[:, kc, g0:g0 + gw],
                             start=(kc == 0), stop=(kc == 7), reads=["XT", "wgc%d" % sl], writes=[gk_],
                             signal=(kc == 7))
                    for kc in range(8):
                        p.mm(ub[:, 0:gw], wuc[sl][:, kc, fb * 128:(fb + 1) * 128], XT[:, kc, g0:g0 + gw],
                             start=(kc == 0), stop=(kc == 7), reads=["XT", "wuc%d" % sl], writes=[uk_],
                             signal=(kc == 7))
                    p.act(sg6[fb % 2][:, 0:gw], gb[:, 0:gw], AF.Silu, reads=[gk_], writes=["sg6_%d" % (fb % 2)])
                    p.tt("dve", hb6[:, fb, 0:gw], sg6[fb % 2][:, 0:gw], ub[:, 0:gw], ALU.mult,
                         reads=["sg6_%d" % (fb % 2), uk_], writes=[hk6])
                for j in range(gw // 128):
                    ti = g0 // 128 + j
                    for hf in range(2):
                        bank = PB[4 + hf]
                        bkey = "pb%d" % (4 + hf)
                        for fb in range(4):
                            p.mm(bank[:], hb6[:, fb, j * 128:(j + 1) * 128], wdc[sl][:, fb, hf * 512:(hf + 1) * 512],
                                 start=(fb == 0), stop=(fb == 3), reads=[hk6, "wdc%d" % sl], writes=[bkey],
                                 signal=(fb == 3))
                        ysl = Y[:, ti, hf * 512:(hf + 1) * 512]
                        if cf == 0:
                            if hf == 0:
                                p.cp("act", ysl, bank[:], reads=[bkey], writes=["Y"])
                            else:
                                p.cp("pool" if False else "dve", ysl, bank[:], reads=[bkey], writes=["Y"])
                        else:
                            p.tt("dve", ysl, ysl, bank[:], ALU.add, reads=["Y", bkey], writes=["Y"])
                gcount += 1
            if cf == NCH - 1:
                p.ld("sp", Yslot[e_ * CAP:(e_ + 1) * CAP, :].rearrange("(i p) d -> p i d", p=128), Y[:],
                     reads=["Y"], writes=["Yslot"])
        p.phase_end()

    if stop_phase >= 7:
        p.phase_begin()
        p.ld("sp", gbc[:], lng[3:4, :].partition_broadcast(128), writes=["gbc"])
        p.ld("sp", bbc[:], lnb[3:4, :].partition_broadcast(128), writes=["bbc"])
        Y1 = [p.sb("Y1_%d" % i, [128, D], F32) for i in range(2)]
        Y2 = [p.sb("Y2_%d" % i, [128, D], F32) for i in range(2)]
        xf7 = [p.sb("x7f%d" % i, [128, D], F32) for i in range(2)]
        y7 = p.sb("y7", [128, D], F32)
        ob = [p.sb("ob%d" % i, [128, D], F32) for i in range(2)]

        def ld7(t):
            sl = t % 2
            p.ld("sp", xf7[sl][:], h_c[t * 128:(t + 1) * 128, :], reads=["h_c_d%d" % t], writes=["x7f%d" % sl])
            for k2, Yk in enumerate((Y1, Y2)):
                p.dma("pool", lambda e, t=t, k2=k2, Yk=Yk, sl=sl: e.indirect_dma_start(
                    out=Yk[sl][:], out_offset=None, in_=Yslot[:, :],
                    in_offset=bass.IndirectOffsetOnAxis(ap=rpos[:, t, k2:k2 + 1], axis=0)),
                    reads=["Yslot", "Yzero", "rpos"], writes=["Y%d_%d" % (k2 + 1, sl)])

        ld7(0)
        for t in range(NT):
            sl = t % 2
            if t + 1 < NT:
                ld7(t + 1)
            p.ts("dve", y7[:], Y1[sl][:], rinfo[:, t, 0:1], None, ALU.mult, reads=["Y1_%d" % sl, "rinfo"],
                 writes=["y7"])
            p.stt(y7[:], Y2[sl][:], rinfo[:, t, 1:2], y7[:], ALU.mult, ALU.add, reads=["Y2_%d" % sl, "rinfo", "y7"],
                  writes=["y7"])
            p.stt(y7[:], xf7[sl][:], ALPHA, y7[:], ALU.mult, ALU.add, reads=["x7f%d" % sl, "y7"], writes=["y7"])
            layernorm_tile(p, y7[:], ob[sl][:], gbc[:], bbc[:], sm, "y7", "ob%d" % sl, extra_reads=["gbc", "bbc"])
            p.ld("sp", out_d[t * 128:(t + 1) * 128, :], ob[sl][:], reads=["ob%d" % sl], writes=["out%d" % t])
        p.wait_all("sp", ["out%d" % t for t in range(NT)])
        p.phase_end(final=True)
        return nc

    p.phase_begin()
    dbg = p.sb("dbg", [128, D], F32)
    for t in range(n_dbg):
        p.ld("sp", dbg[:], dbg_src[t * 128:(t + 1) * 128, :], writes=["dbg"])
        p.ld("sp", out_d[t * 128:(t + 1) * 128, :], dbg[:], reads=["dbg"], writes=["out%d" % t])
    p.wait_all("sp", ["out%d" % t for t in range(n_dbg)])
    p.phase_end(final=True)
    return nc


def make_consts():
    c = np.zeros((128, 1024), np.float32)
    c[:, 0:128] = np.eye(128, dtype=np.float32)
    s = np.arange(128)[:, None]
    t = np.arange(128)[None, :]
    c[:, 128:256] = (s <= t).astype(np.float32)
    for h in range(4):
        c[h, 256 + h * 128:256 + (h + 1) * 128] = 1.0
    return c


def make_consts2():
    c = np.zeros((128, 4096), np.float32)
    k = np.arange(128)[:, None]
    q = np.arange(512)[None, :]
    for r in range(4):
        c[:, r * 512:(r + 1) * 512] = ((128 * r + k) < q).astype(np.float32)
    kk = np.arange(128)[None, :]
    c[:, 2048:2176] = (k > kk).astype(np.float32)
    c[:, 2176:2304] = (k < kk).astype(np.float32)
    c[:, 2304:2432] = (k >= kk).astype(np.float32)
    c[:, 2432] = (np.arange(128) >= PAD).astype(np.float32)
    c[:, 2440:2448] = (np.arange(NE) * CAP).astype(np.float32)[None, :]
    c[:, 2448] = (NE * CAP + np.arange(128)).astype(np.float32)
    return c


def make_in_maps(inputs):
    x = np.asarray(inputs["x"], np.float32)
    meta = np.asarray(inputs["meta"], np.float32)
    bg = np.asarray(inputs["b_gate_a"], np.float32)[0]
    shared = {
        "w_in": np.ascontiguousarray(inputs["w_in_a"][0]),
        "bgate": np.ascontiguousarray(np.stack([bg[0:4], bg[4:8]], axis=1)),
        "norm_pk": np.ascontiguousarray(np.asarray(inputs["norm_a"], np.float32)[0].reshape(8, 128).T),
        "w_out": np.ascontiguousarray(inputs["w_out_a"][0]),
        "lng": np.ascontiguousarray(np.asarray(inputs["ln_g"], np.float32).reshape(4, D)),
        "lnb": np.ascontiguousarray(np.asarray(inputs["ln_b"], np.float32).reshape(4, D)),
        "consts": make_consts(),
        "consts2": make_consts2(),
        "w_gd": np.ascontiguousarray(inputs["w_gate_d"][0]),
        "w_ud": np.ascontiguousarray(inputs["w_up_d"][0]),
        "w_dd": np.ascontiguousarray(inputs["w_down_d"][0]),
        "w_kv": np.ascontiguousarray(inputs["w_kv"]),
        "w_q": np.ascontiguousarray(inputs["w_q_b"][0]),
        "w_o": np.ascontiguousarray(inputs["w_o_b"][0]),
        "w_r": np.ascontiguousarray(inputs["w_router"][0]),
        "w_ge": np.ascontiguousarray(inputs["w_gate_e"][0]),
        "w_ue": np.ascontiguousarray(inputs["w_up_e"][0]),
        "w_de": np.ascontiguousarray(inputs["w_down_e"][0]),
    }
    maps = []
    for c in range(NCORES):
        hin = np.zeros((2, RS, D), np.float32)
        for s in range(2):
            hin[s, PAD:128] = meta
            hin[s, 128:] = x[2 * c + s]
        m = dict(shared)
        m["hin"] = hin.reshape(R, D)
        maps.append(m)
    return maps


def kernel(**inputs):
    nc = build()
    maps = make_in_maps(inputs)
    res = run_bass_kernel_spmd(nc, maps, core_ids=list(range(NCORES)))
    out = np.zeros((16, SEQ, D), np.float32)
    for c in range(NCORES):
        o = res.results[c]["out"].reshape(2, RS, D)
        out[2 * c:2 * c + 2] = o[:, 128:, :]
    return out
```

```python
import numpy as np
from contextlib import ExitStack
import concourse.bass as bass
import concourse.mybir as mybir
from concourse.bass_utils import run_bass_kernel_spmd

F32 = mybir.dt.float32
BF16 = mybir.dt.bfloat16
I32 = mybir.dt.int32
U32 = mybir.dt.uint32
AF = mybir.ActivationFunctionType
ALU = mybir.AluOpType
AX = mybir.AxisListType

NDSEM = 8
NCORES = 8
D = 1024
SEQ = 4096
NMETA = 16
TPS = 33
RS = TPS * 128
R = 2 * RS
NT = 2 * TPS
PAD = 112
ALPHA = 4.0 ** 0.25
EPS = 1e-5
DFF = 2816
NE = 8
DFE = 3584
CAP = 2560


class Prog:
    ENGS = ("pe", "act", "dve", "pool", "sp")

    def __init__(self, nc):
        self.nc = nc
        self.ges = ExitStack()
        self.es = self.ges
        self.stream = {e: [] for e in self.ENGS}
        self.cnt = {e: 0 for e in self.ENGS}
        self.waited = {e: {} for e in self.ENGS}
        self.res_w = {}
        self.res_r = {}
        self.dq_n = {q: 0 for q in ("sp", "act", "pool")}
        self.sems = {}
        for e in self.ENGS:
            self.sems[("e", e)] = self.ges.enter_context(nc.semaphore("s_e_" + e))
        for q in ("sp", "act", "pool"):
            for i in range(NDSEM):
                self.sems[("d", q, i)] = self.ges.enter_context(nc.semaphore("s_d_%s_%d" % (q, i)))

    def phase_begin(self):
        self.es = ExitStack()

    def barrier(self):
        for e in self.ENGS:
            waits = {}
            for e2 in self.ENGS:
                if e2 != e and self.cnt[e2] > 0:
                    self._need(e, (("e", e2), self.cnt[e2], e2), waits)
            for q in ("sp", "act", "pool"):
                n = self.dq_n[q]
                for slot in range(NDSEM):
                    k = (n - slot + NDSEM - 1) // NDSEM
                    if k > 0:
                        self._need(e, (("d", q, slot), 16 * k, "dma"), waits)
            self.stream[e].append((None, waits, None, 0))

    def phase_end(self, final=False):
        self.barrier()
        self.emit_block()
        if self.es is not self.ges:
            self.es.close()
        self.es = self.ges
        if final:
            self.ges.close()

    def sb(self, name, shape, dtype):
        self.nalloc = getattr(self, "nalloc", 0) + 1
        return self.es.enter_context(self.nc.sbuf_tensor("%s_%d" % (name, self.nalloc), list(shape), dtype))

    def ps(self, name, shape, dtype):
        return self.es.enter_context(self.nc.psum_tensor(name, list(shape), dtype))

    def dram(self, name, shape, dtype, kind="Internal"):
        return self.nc.dram_tensor(name, list(shape), dtype, kind=kind).ap()

    skip_self = False

    def _need(self, eng, tok, waits):
        if tok is None:
            return
        semkey, val, src = tok
        if src == "pe" and eng == "pe":
            return
        if self.skip_self and src == eng:
            return
        if self.waited[eng].get(semkey, 0) >= val:
            return
        self.waited[eng][semkey] = val
        waits[semkey] = max(waits.get(semkey, 0), val)

    def _deps(self, eng, reads, writes):
        waits = {}
        for r in reads:
            self._need(eng, self.res_w.get(r), waits)
        for w in writes:
            self._need(eng, self.res_w.get(w), waits)
            for t in self.res_r.get(w, ()):
                self._need(eng, t, waits)
        return waits

    def _record(self, tok, reads, writes):
        for r in reads:
            self.res_r.setdefault(r, []).append(tok)
        for w in writes:
            self.res_w[w] = tok
            self.res_r[w] = []

    def op(self, eng, fn, reads=(), writes=(), signal=True):
        waits = self._deps(eng, reads, writes)
        if signal:
            self.cnt[eng] += 1
            tok = (("e", eng), self.cnt[eng], eng)
        else:
            tok = (("e", eng), self.cnt[eng] + 1, eng)
        self._record(tok, reads, writes)
        self.stream[eng].append((fn, waits, ("e", eng) if signal else None, 1))

    def dma(self, q, fn, reads=(), writes=()):
        waits = self._deps(q, reads, writes)
        i = self.dq_n[q]
        self.dq_n[q] += 1
        slot = i % NDSEM
        semkey = ("d", q, slot)
        prev = 16 * (i // NDSEM)
        if prev > 0 and self.waited[q].get(semkey, 0) < prev:
            self.waited[q][semkey] = prev
            waits[semkey] = max(waits.get(semkey, 0), prev)
        tok = (semkey, prev + 16, "dma")
        self._record(tok, reads, writes)
        self.stream[q].append((fn, waits, semkey, 16))

    def wait_all(self, eng, keys):
        waits = {}
        for k in keys:
            self._need(eng, self.res_w.get(k), waits)
        self.stream[eng].append((None, waits, None, 0))

    def mm(self, out, lhsT, rhs, start=True, stop=True, reads=(), writes=(), signal=True):
        self.op("pe", lambda e: e.matmul(out, lhsT, rhs, start=start, stop=stop),
                reads, writes, signal)

    def tr(self, out, in_, ident, reads=(), writes=(), signal=True):
        self.op("pe", lambda e: e.transpose(out, in_, ident), reads, writes, signal)

    def act(self, out, in_, func, bias=None, scale=None, reads=(), writes=()):
        kw = {}
        if bias is not None:
            kw["bias"] = bias
        if scale is not None:
            kw["scale"] = scale
        self.op("act", lambda e: e.activation(out=out, in_=in_, func=func, **kw), reads, writes)

    def tt(self, eng, out, in0, in1, op, reads=(), writes=()):
        self.op(eng, lambda e: e.tensor_tensor(out=out, in0=in0, in1=in1, op=op), reads, writes)

    def ts(self, eng, out, in0, s1, s2, op0, op1=None, reads=(), writes=()):
        if op1 is None:
            self.op(eng, lambda e: e.tensor_scalar(out=out, in0=in0, scalar1=s1, scalar2=None, op0=op0),
                    reads, writes)
        else:
            self.op(eng, lambda e: e.tensor_scalar(out=out, in0=in0, scalar1=s1, scalar2=s2, op0=op0, op1=op1),
                    reads, writes)

    def stt(self, out, in0, scalar, in1, op0, op1, reads=(), writes=()):
        self.op("dve", lambda e: e.scalar_tensor_tensor(out=out, in0=in0, scalar=scalar, in1=in1,
                                                        op0=op0, op1=op1), reads, writes)

    def cp(self, eng, out, in_, reads=(), writes=()):
        if eng == "act":
            self.op("act", lambda e: e.copy(out=out, in_=in_), reads, writes)
        else:
            self.op(eng, lambda e: e.tensor_copy(out=out, in_=in_), reads, writes)

    def ld(self, q, out, in_, reads=(), writes=()):
        self.dma(q, lambda e: e.dma_start(out=out, in_=in_), reads, writes)

    def emit_block(self):
        nc = self.nc
        with nc.Block() as block:
            handles = {"pe": block.tensor, "act": block.scalar, "dve": block.vector,
                       "pool": block.gpsimd, "sp": block.sync}

            def mk(e):
                def body(engh):
                    for fn, waits, sk, inc in self.stream[e]:
                        for k, v in waits.items():
                            engh.wait_ge(self.sems[k], v)
                        if fn is None:
                            continue
                        ins = fn(engh)
                        if sk is not None:
                            ins.then_inc(self.sems[sk], inc)
                return body

            for e in self.ENGS:
                if self.stream[e]:
                    handles[e](mk(e))
        self.stream = {e: [] for e in self.ENGS}


def load_w_bf(p, name, w_ap, K, N, key):
    t = p.sb(name, [128, K // 128, N], BF16)
    src = w_ap.rearrange("(k p) n -> p k n", p=128)
    c0 = 0
    while c0 < N:
        c1 = min(N, c0 + 2048)
        p.ld("pool", t[:, :, c0:c1], src[:, :, c0:c1], writes=[key])
        c0 = c1
    return t


def layernorm_tile(p, y, out, gbc, bbc, sm, key_y, key_out, extra_reads=()):
    st = sm[:, 0:12].rearrange("p (a b) -> p a b", a=2)
    for hf in range(2):
        p.op("dve", lambda e, hf=hf: e.bn_stats(out=st[:, hf, :], in_=y[:, hf * 512:(hf + 1) * 512]),
             reads=[key_y], writes=["sm_ln"])
    p.op("dve", lambda e: e.bn_aggr(out=sm[:, 12:14], in_=sm[:, 0:12]), reads=["sm_ln"], writes=["sm_ln"])
    p.act(sm[:, 14:15], sm[:, 13:14], AF.Sqrt, bias=sm[:, 20:21], reads=["sm_ln", "sm_eps"], writes=["sm_ln"])
    p.op("dve", lambda e: e.reciprocal(out=sm[:, 15:16], in_=sm[:, 14:15]), reads=["sm_ln"], writes=["sm_ln"])
    p.ts("dve", sm[:, 16:17], sm[:, 12:13], sm[:, 15:16], -1.0, ALU.mult, ALU.mult,
         reads=["sm_ln"], writes=["sm_ln"])
    p.act(out, y, AF.Identity, bias=sm[:, 16:17], scale=sm[:, 15:16], reads=[key_y, "sm_ln"], writes=[key_out])
    p.tt("dve", out, out, gbc, ALU.mult, reads=[key_out] + list(extra_reads), writes=[key_out])
    p.tt("dve", out, out, bbc, ALU.add, reads=[key_out], writes=[key_out])


def build(stop_after=99, stop_phase=7):
    nc = bass.Bass("TRN2", target_bir_lowering=False)
    p = Prog(nc)
    din = lambda n, s: nc.dram_tensor(n, list(s), F32, kind="ExternalInput").ap()
    hin = din("hin", [R, D])
    w_in = din("w_in", [D, 3080])
    bgate = din("bgate", [4, 2])
    norm_pk = din("norm_pk", [128, 8])
    w_out = din("w_out", [D, D])
    lng = din("lng", [4, D])
    lnb = din("lnb", [4, D])
    consts = din("consts", [128, 1024])
    consts2 = din("consts2", [128, 4096])
    w_gd = din("w_gd", [D, DFF])
    w_ud = din("w_ud", [D, DFF])
    w_dd = din("w_dd", [DFF, D])
    w_kv = din("w_kv", [D, 2 * D])
    w_q = din("w_q", [D, D])
    w_o = din("w_o", [D, D])
    w_r = din("w_r", [D, NE])
    w_ge = din("w_ge", [NE, D, DFE])
    w_ue = din("w_ue", [NE, D, DFE])
    w_de = din("w_de", [NE, DFE, D])
    out_d = nc.dram_tensor("out", [R, D], F32, kind="ExternalOutput").ap()
    h_a = p.dram("h_a", [R, D], F32)
    h_b = p.dram("h_b", [R, D], F32)
    h_c = p.dram("h_c", [R, D], F32)
    KT_d = p.dram("KT_d", [8, 128, R], BF16)
    QT_d = p.dram("QT_d", [8, 128, R], BF16)
    V_d = p.dram("V_d", [R, D], BF16)
    OT_d = p.dram("OT_d", [8, 128, R], BF16)
    NSLOT = NE * CAP
    Xslot = p.dram("Xslot", [NSLOT + 128, D], BF16)
    Yslot = p.dram("Yslot", [NSLOT + 128, D], F32)

    cst = p.sb("cst", [128, 1024], F32)
    p.ld("sp", cst[:], consts, writes=["cst"])
    ident_bf = p.sb("ident_bf", [128, 128], BF16)
    p.ld("pool", ident_bf[:], consts[:, 0:128], writes=["ident_bf"])
    ones_bf = p.sb("ones_bf", [128, 8], BF16)
    p.op("dve", lambda e: e.memset(ones_bf[:], 1.0), writes=["ones_bf"])
    ident_f = cst[:, 0:128]
    cmask = cst[:, 128:256]
    sel = cst[0:4, 256:768].rearrange("p (h n) -> p h n", h=4)
    bg = p.sb("bg", [4, 4], F32)
    p.ld("sp", bg[:, 0:2], bgate, writes=["bg"])
    p.ts("dve", bg[:, 2:3], bg[:, 1:2], -1.0, None, ALU.mult, reads=["bg"], writes=["bg"])
    p.op("dve", lambda e: e.memset(bg[:, 3:4], 0.0), reads=["bg"], writes=["bg"])
    ones4 = p.sb("ones4", [4, 128], F32)
    p.op("dve", lambda e: e.memset(ones4[:], 1.0), writes=["ones4"])
    sm = p.sb("sm", [128, 32], F32)
    p.op("dve", lambda e: e.memset(sm[:, 20:21], EPS), writes=["sm_eps"])
    gbc = p.sb("gbc", [128, D], F32)
    bbc = p.sb("bbc", [128, D], F32)

    tpb = p.ps("tpb", [128, 8, 128], BF16)
    PB = [p.ps("pb%d" % i, [128, 512], F32) for i in range(7)]
    pA, pB, pC, pD, pE, pF, pG = PB

    p.phase_begin()
    if True:
        p.ld("sp", gbc[:], lng[0:1, :].partition_broadcast(128), writes=["gbc"])
        p.ld("sp", bbc[:], lnb[0:1, :].partition_broadcast(128), writes=["bbc"])
        win = load_w_bf(p, "win", w_in, D, 3080, "win")
        wout = load_w_bf(p, "wout", w_out, D, D, "wout")
        npk = p.sb("npk", [128, 8], F32)
        p.ld("sp", npk[:], norm_pk, writes=["npk"])
        for kc in range(8):
            p.ts("pool", wout[:, kc, :], wout[:, kc, :], npk[:, kc:kc + 1], None, ALU.mult,
                 reads=["wout", "npk"], writes=["wout"])
        xbf = [p.sb("xbf%d" % i, [128, D], BF16) for i in range(2)]
        xf = [p.sb("xf%d" % i, [128, D], F32) for i in range(2)]
        hT = [p.sb("hT%d" % i, [128, 8, 128], BF16) for i in range(2)]
        G = [p.sb("G%d" % i, [4, 12, 128], F32) for i in range(2)]
        Wbc = p.sb("Wbc", [128, 4, 128], F32)
        tok = p.sb("tok", [128, 12], F32)
        qT = p.sb("qT", [128, 4, 128], BF16)
        kT = p.sb("kT", [128, 4, 128], BF16)
        wk = p.sb("wk", [128, 4, 128], BF16)
        V = p.sb("V", [128, 4, 256], BF16)
        osig = p.sb("osig", [128, D], BF16)
        PT = p.sb("PT", [128, 4, 128], BF16)
        Cf = p.sb("Cf", [128, 4, 256], F32)
        nf = p.sb("nf", [128, 4], F32)
        Cbf = p.sb("Cbf", [128, 4, 256], BF16)
        nbf = p.sb("nbf", [128, 4], BF16)
        nrm = p.sb("nrm", [128, 64], F32)
        hn = p.sb("hn", [128, D], F32)
        outtok = p.sb("outtok", [128, D], BF16)
        outT = p.sb("outT", [128, 8, 128], BF16)
        y = p.sb("y", [128, D], F32)
        ha = [p.sb("ha%d" % i, [128, D], F32) for i in range(2)]
        KSC = 128.0 ** -0.5

        def stage_load(t):
            sl = t % 2
            rows = slice(t * 128, (t + 1) * 128)
            p.ld("pool", xbf[sl][:], hin[rows, :], writes=["xbf%d" % sl])
            p.ld("sp", xf[sl][:], hin[rows, :], writes=["xf%d" % sl])

        stage_load(0)
        for t in range(NT):
            if t >= stop_after:
                break
            sl = t % 2
            c = t % TPS
            first = c == 0
            if t + 1 < NT:
                stage_load(t + 1)
            g = G[sl]
            gp = G[1 - sl]
            gk = "G%d" % sl
            gpk = "G%d" % (1 - sl)
            for kc in range(8):
                p.tr(tpb[:, kc, :], xbf[sl][:, kc * 128:(kc + 1) * 128], ident_bf[:],
                     reads=["xbf%d" % sl, "ident_bf"], writes=["tpb"], signal=(kc == 7))
            p.cp("act", hT[sl][:], tpb[:], reads=["tpb"], writes=["hT%d" % sl])
            hk = "hT%d" % sl
            for gi in range(2):
                for kc in range(8):
                    p.mm(pE[0:4, gi * 128:(gi + 1) * 128], win[:, kc, 3072 + 4 * gi:3076 + 4 * gi],
                         hT[sl][:, kc, :], start=(kc == 0), stop=(kc == 7),
                         reads=[hk, "win"], writes=["pE_g"], signal=(kc == 7))
            p.act(g[:, 0, :], pE[0:4, 0:128], AF.Identity, bias=bg[:, 0:1], reads=["pE_g", "bg"], writes=[gk])
            p.act(g[:, 10, :], pE[0:4, 128:256], AF.Exp, bias=bg[:, 2:3], scale=-1.0,
                  reads=["pE_g", "bg"], writes=[gk])
            p.act(g[:, 11, :], g[:, 10, :], AF.Ln, bias=1.0, reads=[gk], writes=[gk])
            p.ts("dve", g[:, 1, :], g[:, 11, :], -1.0, None, ALU.mult, reads=[gk], writes=[gk])
            if first:
                p.op("dve", lambda e, g=g: e.memset(g[:, 0, 0:PAD], -30000.0), reads=[gk], writes=[gk])
                p.op("dve", lambda e, g=g: e.memset(g[:, 1, 0:PAD], 0.0), reads=[gk], writes=[gk])
                Bi, mi, a0 = bg[:, 3:4], bg[:, 3:4], bg[:, 3:4]
            else:
                Bi, mi, a0 = gp[:, 2, 127:128], gp[:, 3, 127:128], gp[:, 4, 127:128]
            p.op("dve", lambda e, g=g, Bi=Bi: e.tensor_tensor_scan(
                out=g[:, 2, :], data0=ones4[:], data1=g[:, 1, :], initial=Bi, op0=ALU.mult, op1=ALU.add),
                reads=[gk, gpk, "ones4", "bg"], writes=[gk])
            p.op("dve", lambda e, g=g, mi=mi: e.tensor_tensor_scan(
                out=g[:, 3, :], data0=g[:, 1, :], data1=g[:, 0, :], initial=mi, op0=ALU.add, op1=ALU.max),
                reads=[gk, gpk, "bg"], writes=[gk])
            p.tt("dve", g[:, 4, :], g[:, 2, :], g[:, 3, :], ALU.subtract, reads=[gk], writes=[gk])
            p.tt("dve", g[:, 6, :], g[:, 0, :], g[:, 2, :], ALU.subtract, reads=[gk], writes=[gk])
            p.ts("dve", g[:, 10, :], g[:, 4, :], a0, None, ALU.subtract, reads=[gk, gpk, "bg"], writes=[gk])
            p.act(g[:, 5, :], g[:, 10, :], AF.Exp, reads=[gk], writes=[gk])
            p.act(g[:, 7, :], g[:, 6, :], AF.Exp, bias=a0, reads=[gk, gpk, "bg"], writes=[gk])
            p.act(g[:, 8, :], g[:, 6, :], AF.Exp, bias=g[:, 4, 127:128], reads=[gk], writes=[gk])
            p.act(g[:, 9, :], g[:, 3, :], AF.Exp, scale=-1.0, reads=[gk], writes=[gk])
            for h in range(4):
                p.mm(pD[:, h * 128:(h + 1) * 128], sel[:, h, :], g[:, 5, :], reads=[gk, "cst"],
                     writes=["pD"], signal=(h == 3))
            p.cp("act", Wbc[:].rearrange("p h n -> p (h n)"), pD[:], reads=["pD"], writes=["Wbc"])
            for j in range(3):
                p.tr(pE[:, 256 + 4 * j:260 + 4 * j], g[:, 7 + j, :], ident_f[0:4, 0:4],
                     reads=[gk, "cst"], writes=["pE_t"], signal=(j == 2))
            p.cp("dve", tok[:], pE[:, 256:268], reads=["pE_t"], writes=["tok"])
            for h in range(4):
                for kc in range(8):
                    p.mm(pA[:, h * 128:(h + 1) * 128], win[:, kc, h * 128:(h + 1) * 128], hT[sl][:, kc, :],
                         start=(kc == 0), stop=(kc == 7), reads=[hk, "win"], writes=["pA"],
                         signal=(h == 3 and kc == 7))
            p.tt("dve", qT[:].rearrange("p h n -> p (h n)"), pA[:], Wbc[:].rearrange("p h n -> p (h n)"),
                 ALU.mult, reads=["pA", "Wbc"], writes=["qT"])
            for h in range(4):
                for kc in range(8):
                    p.mm(pA[:, h * 128:(h + 1) * 128], win[:, kc, 512 + h * 128:512 + (h + 1) * 128],
                         hT[sl][:, kc, :], start=(kc == 0), stop=(kc == 7), reads=[hk, "win"], writes=["pA"],
                         signal=(h == 3 and kc == 7))
            p.op("act", lambda e: e.mul(out=kT[:].rearrange("p h n -> p (h n)"), in_=pA[:], mul=KSC),
                 reads=["pA"], writes=["kT"])
            banks = [pB, pC]
            bk = ["pB", "pC"]

            def tokmm(bi, col0):
                for kc in range(8):
                    p.mm(banks[bi][:], hT[sl][:, kc, :], win[:, kc, col0:col0 + 512], start=(kc == 0),
                         stop=(kc == 7), reads=[hk, "win"], writes=[bk[bi]], signal=(kc == 7))

            tokmm(0, 512)
            for h in range(4):
                p.ts("dve", wk[:, h, :], pB[:, h * 128:(h + 1) * 128], tok[:, 4 + h:5 + h], KSC,
                     ALU.mult, ALU.mult, reads=["pB", "tok"], writes=["wk"])
            tokmm(1, 1024)
            p.cp("act", V[:, 0:2, :].rearrange("p h n -> p (h n)"), pC[:], reads=["pC"], writes=["V"])
            tokmm(0, 1536)
            p.cp("act", V[:, 2:4, :].rearrange("p h n -> p (h n)"), pB[:], reads=["pB"], writes=["V"])
            tokmm(1, 2048)
            p.act(osig[:, 0:512], pC[:], AF.Sigmoid, reads=["pC"], writes=["osig"])
            tokmm(0, 2560)
            p.act(osig[:, 512:1024], pB[:], AF.Sigmoid, reads=["pB"], writes=["osig"])
            if first:
                p.op("dve", lambda e: e.memset(Cf[:], 0.0), writes=["Cf"])
                p.op("dve", lambda e: e.memset(nf[:], 0.0), writes=["nf"])
                p.op("dve", lambda e: e.memset(Cbf[:], 0.0), writes=["Cbf"])
                p.op("dve", lambda e: e.memset(nbf[:], 0.0), writes=["nbf"])
            for h in range(4):
                p.mm(pA[:, h * 128:(h + 1) * 128], kT[:, h, :], qT[:, h, :], reads=["kT", "qT"], writes=["pA"],
                     signal=(h == 3))
            for h in range(4):
                p.stt(PT[:, h, :], pA[:, h * 128:(h + 1) * 128], tok[:, h:h + 1], cmask, ALU.mult, ALU.mult,
                      reads=["pA", "tok", "cst"], writes=["PT"])
            for h in range(4):
                bank = pF if h < 2 else pG
                bkey = "pF" if h < 2 else "pG"
                reg = bank[:, (h % 2) * 256:(h % 2) * 256 + 256]
                p.mm(reg, PT[:, h, :], V[:, h, :], start=True, stop=False, reads=["PT", "V"], writes=[bkey],
                     signal=False)
                p.mm(reg, qT[:, h, :], Cbf[:, h, :], start=False, stop=True, reads=["qT", "Cbf"], writes=[bkey])
                p.mm(pE[:, 272 + h:273 + h], PT[:, h, :], ones_bf[:, 0:1], start=True, stop=False,
                     reads=["PT", "ones_bf"], writes=["pE_d"], signal=False)
                p.mm(pE[:, 272 + h:273 + h], qT[:, h, :], nbf[:, h:h + 1], start=False, stop=True,
                     reads=["qT", "nbf"], writes=["pE_d"])
            p.act(nrm[:, 0:4], pE[:, 272:276], AF.Abs, reads=["pE_d"], writes=["nrm"])
            p.tt("dve", nrm[:, 0:4], nrm[:, 0:4], tok[:, 8:12], ALU.max, reads=["nrm", "tok"], writes=["nrm"])
            p.op("dve", lambda e: e.reciprocal(out=nrm[:, 0:4], in_=nrm[:, 0:4]), reads=["nrm"], writes=["nrm"])
            for h in range(4):
                bank = pF if h < 2 else pG
                bkey = "pF" if h < 2 else "pG"
                reg = bank[:, (h % 2) * 256:(h % 2) * 256 + 256]
                p.op("dve", lambda e, h=h, reg=reg: e.bn_stats(out=nrm[:, 4 + 6 * h:10 + 6 * h], in_=reg),
                     reads=[bkey, "nrm"], writes=["nrm"])
                p.op("dve", lambda e, h=h: e.bn_aggr(out=nrm[:, 28 + 2 * h:30 + 2 * h],
                                                     in_=nrm[:, 4 + 6 * h:10 + 6 * h]),
                     reads=["nrm"], writes=["nrm"])
            mv = nrm[:, 28:36].rearrange("p (h two) -> p h two", two=2)
            p.tt("dve", nrm[:, 36:40], nrm[:, 0:4], nrm[:, 0:4], ALU.mult, reads=["nrm"], writes=["nrm"])
            p.tt("dve", nrm[:, 36:40], nrm[:, 36:40], mv[:, :, 1], ALU.mult, reads=["nrm"], writes=["nrm"])
            p.act(nrm[:, 36:40], nrm[:, 36:40], AF.Sqrt, bias=sm[:, 20:21], reads=["nrm", "sm_eps"],
                  writes=["nrm"])
            p.op("dve", lambda e: e.reciprocal(out=nrm[:, 36:40], in_=nrm[:, 36:40]), reads=["nrm"],
                 writes=["nrm"])
            p.tt("dve", nrm[:, 40:44], nrm[:, 36:40], nrm[:, 0:4], ALU.mult, reads=["nrm"], writes=["nrm"])
            p.tt("dve", nrm[:, 44:48], nrm[:, 40:44], mv[:, :, 0], ALU.mult, reads=["nrm"], writes=["nrm"])
            p.ts("dve", nrm[:, 44:48], nrm[:, 44:48], -1.0, None, ALU.mult, reads=["nrm"], writes=["nrm"])
            for h in range(4):
                bank = pF if h < 2 else pG
                bkey = "pF" if h < 2 else "pG"
                reg = bank[:, (h % 2) * 256:(h % 2) * 256 + 256]
                p.act(hn[:, h * 256:(h + 1) * 256], reg, AF.Identity, bias=nrm[:, 44 + h:45 + h],
                      scale=nrm[:, 40 + h:41 + h], reads=[bkey, "nrm"], writes=["hn"])
            p.tt("dve", outtok[:], hn[:], osig[:], ALU.mult, reads=["hn", "osig"], writes=["outtok"])
            for h in range(4):
                bank = pF if h < 2 else pG
                bkey = "pF" if h < 2 else "pG"
                reg = bank[:, (h % 2) * 256:(h % 2) * 256 + 256]
                p.mm(reg, wk[:, h, :], V[:, h, :], reads=["wk", "V"], writes=[bkey])
                p.mm(pE[:, 280 + h:281 + h], wk[:, h, :], ones_bf[:, 0:1], reads=["wk", "ones_bf"],
                     writes=["pE_n"])
                dec = Wbc[:, h, 127:128]
                p.stt(Cf[:, h, :], Cf[:, h, :], dec, reg, ALU.mult, ALU.add, reads=["Cf", "Wbc", bkey],
                      writes=["Cf"])
                p.stt(nf[:, h:h + 1], nf[:, h:h + 1], dec, pE[:, 280 + h:281 + h], ALU.mult, ALU.add,
                      reads=["nf", "Wbc", "pE_n"], writes=["nf"])
            p.cp("act", Cbf[:].rearrange("p h n -> p (h n)"), Cf[:].rearrange("p h n -> p (h n)"),
                 reads=["Cf"], writes=["Cbf"])
            p.cp("act", nbf[:], nf[:], reads=["nf"], writes=["nbf"])
            for kc in range(8):
                p.tr(tpb[:, kc, :], outtok[:, kc * 128:(kc + 1) * 128], ident_bf[:],
                     reads=["outtok", "ident_bf"], writes=["tpb"], signal=(kc == 7))
            p.cp("act", outT[:], tpb[:], reads=["tpb"], writes=["outT"])
            for hf in range(2):
                bi = 1 - hf
                for kc in range(8):
                    p.mm(banks[bi][:], outT[:, kc, :], wout[:, kc, hf * 512:(hf + 1) * 512], start=(kc == 0),
                         stop=(kc == 7), reads=["outT", "wout"], writes=[bk[bi]], signal=(kc == 7))
                p.stt(y[:, hf * 512:(hf + 1) * 512], xf[sl][:, hf * 512:(hf + 1) * 512], ALPHA, banks[bi][:],
                      ALU.mult, ALU.add, reads=["xf%d" % sl, bk[bi]], writes=["y"])
            layernorm_tile(p, y[:], ha[sl][:], gbc[:], bbc[:], sm, "y", "ha%d" % sl, extra_reads=["gbc", "bbc"])
            p.ld("sp", h_a[t * 128:(t + 1) * 128, :], ha[sl][:], reads=["ha%d" % sl], writes=["h_a_d%d" % t])

    p.phase_end()
    dbg_src = h_a
    n_dbg = min(NT, stop_after)

    def load_c2(p):
        c2 = p.sb("c2", [128, 4096], F32)
        p.ld("sp", c2[:], consts2, writes=["c2"])
        return c2

    def transpose_rows(p, src_bf, dst, col0, rk, wk_):
        for kc in range(8):
            p.tr(tpb[:, kc, :], src_bf[:, kc * 128:(kc + 1) * 128], ident_bf[:],
                 reads=[rk, "ident_bf"], writes=["tpb"], signal=(kc == 7))
        p.cp("act", dst[:, :, col0:col0 + 128], tpb[:], reads=["tpb"], writes=[wk_])

    if stop_phase >= 2:
        p.phase_begin()
        p.ld("sp", gbc[:], lng[1:2, :].partition_broadcast(128), writes=["gbc"])
        p.ld("sp", bbc[:], lnb[1:2, :].partition_broadcast(128), writes=["bbc"])
        wg = load_w_bf(p, "wg", w_gd, D, DFF, "wg")
        wu = load_w_bf(p, "wu", w_ud, D, DFF, "wu")
        wd = load_w_bf(p, "wd", w_dd, DFF, D, "wd")
        NF = DFF // 128
        xbf2 = [p.sb("x2bf%d" % i, [128, 2, D], BF16) for i in range(2)]
        xf2 = [p.sb("x2f%d" % i, [128, D], F32) for i in range(2)]
        hT2 = p.sb("hT2", [128, 8, 256], BF16)
        HT = p.sb("HT", [128, NF, 256], BF16)
        sg = [p.sb("sg%d" % i, [128, 256], F32) for i in range(2)]
        y2 = p.sb("y2", [128, D], F32)
        hb = [p.sb("hb%d" % i, [128, D], F32) for i in range(2)]
        NG = R // 256

        def ld2(g):
            sl = g % 2
            p.ld("pool", xbf2[sl][:], h_a[g * 256:(g + 1) * 256, :].rearrange("(j p) d -> p j d", p=128),
                 reads=["h_a_d%d" % (2 * g), "h_a_d%d" % (2 * g + 1)], writes=["x2bf%d" % sl])

        ld2(0)
        for g in range(NG):
            sl = g % 2
            if g + 1 < NG:
                ld2(g + 1)
            for j in range(2):
                transpose_rows(p, xbf2[sl][:, j, :], hT2, j * 128, "x2bf%d" % sl, "hT2")
            for f in range(NF):
                gb, ub = PB[f % 2], PB[2 + f % 2]
                gk_, uk_ = "pb%d" % (f % 2), "pb%d" % (2 + f % 2)
                for kc in range(8):
                    p.mm(gb[:, 0:256], wg[:, kc, f * 128:(f + 1) * 128], hT2[:, kc, :], start=(kc == 0),
                         stop=(kc == 7), reads=["hT2", "wg"], writes=[gk_], signal=(kc == 7))
                for kc in range(8):
                    p.mm(ub[:, 0:256], wu[:, kc, f * 128:(f + 1) * 128], hT2[:, kc, :], start=(kc == 0),
                         stop=(kc == 7), reads=["hT2", "wu"], writes=[uk_], signal=(kc == 7))
                p.act(sg[f % 2][:], gb[:, 0:256], AF.Silu, reads=[gk_], writes=["sg%d" % (f % 2)])
                p.tt("dve", HT[:, f, :], sg[f % 2][:], ub[:, 0:256], ALU.mult, reads=["sg%d" % (f % 2), uk_],
                     writes=["HT"])
            for j in range(2):
                t = 2 * g + j
                s2 = t % 2
                p.ld("sp", xf2[s2][:], h_a[t * 128:(t + 1) * 128, :], reads=["h_a_d%d" % t],
                     writes=["x2f%d" % s2])
                for hf in range(2):
                    bank = PB[4 + hf]
                    bkey = "pb%d" % (4 + hf)
                    for f in range(NF):
                        p.mm(bank[:], HT[:, f, j * 128:(j + 1) * 128], wd[:, f, hf * 512:(hf + 1) * 512],
                             start=(f == 0), stop=(f == NF - 1), reads=["HT", "wd"], writes=[bkey],
                             signal=(f == NF - 1))
                    p.stt(y2[:, hf * 512:(hf + 1) * 512], xf2[s2][:, hf * 512:(hf + 1) * 512], ALPHA, bank[:],
                          ALU.mult, ALU.add, reads=["x2f%d" % s2, bkey], writes=["y2"])
                layernorm_tile(p, y2[:], hb[s2][:], gbc[:], bbc[:], sm, "y2", "hb%d" % s2,
                               extra_reads=["gbc", "bbc"])
                p.ld("sp", h_b[t * 128:(t + 1) * 128, :], hb[s2][:], reads=["hb%d" % s2], writes=["h_b_d%d" % t])
        p.phase_end()
        dbg_src = h_b
        n_dbg = NT

    if stop_phase >= 3:
        p.phase_begin()
        wkv = load_w_bf(p, "wkv", w_kv, D, 2 * D, "wkv")
        wq = load_w_bf(p, "wq", w_q, D, D, "wq")
        zt = p.sb("zt", [128, 8192], BF16)
        p.op("pool", lambda e: e.memset(zt[:], 0.0), writes=["zt"])
        zrows = 128 * 8
        for i in range(NSLOT // zrows):
            p.ld("sp", Xslot[i * zrows:(i + 1) * zrows, :].rearrange("(p j) d -> p (j d)", p=128), zt[:],
                 reads=["zt"], writes=["Xslot"])
        zf = p.sb("zf", [128, D], F32)
        p.op("pool", lambda e: e.memset(zf[:], 0.0), writes=["zf"])
        p.ld("sp", Yslot[NSLOT:NSLOT + 128, :], zf[:], reads=["zf"], writes=["Yzero"])
        xbf3 = [p.sb("x3bf%d" % i, [128, 4, D], BF16) for i in range(2)]
        hT3 = p.sb("hT3", [128, 8, 512], BF16)
        kq = [p.sb("kq%d" % i, [128, 512], BF16) for i in range(2)]
        vs = [p.sb("vs%d" % i, [128, D], BF16) for i in range(2)]
        QSC = 128.0 ** -0.5
        groups = [(g * 512, 512) for g in range(R // 512)]
        if R % 512:
            groups.append((R - R % 512, R % 512))

        def ld3(gi):
            r0, w = groups[gi]
            sl = gi % 2
            nj = w // 128
            p.ld("pool", xbf3[sl][:, 0:nj, :], h_b[r0:r0 + w, :].rearrange("(j p) d -> p j d", p=128),
                 reads=["h_b_d%d" % (r0 // 128 + j) for j in range(nj)], writes=["x3bf%d" % sl])

        ld3(0)
        cnt3 = 0
        for gi, (r0, w) in enumerate(groups):
            sl = gi % 2
            nj = w // 128
            if gi + 1 < len(groups):
                ld3(gi + 1)
            for j in range(nj):
                transpose_rows(p, xbf3[sl][:, j, :], hT3, j * 128, "x3bf%d" % sl, "hT3")
            for which in range(2):
                for h in range(8):
                    bank = PB[cnt3 % 4]
                    bkey = "pb%d" % (cnt3 % 4)
                    wsrc = wkv if which == 0 else wq
                    for kc in range(8):
                        p.mm(bank[:, 0:w], wsrc[:, kc, h * 128:(h + 1) * 128], hT3[:, kc, 0:w], start=(kc == 0),
                             stop=(kc == 7), reads=["hT3", "wkv", "wq"], writes=[bkey], signal=(kc == 7))
                    ks = kq[cnt3 % 2]
                    kk = "kq%d" % (cnt3 % 2)
                    if which == 0:
                        p.cp("act", ks[:, 0:w], bank[:, 0:w], reads=[bkey], writes=[kk])
                        p.ld("sp", KT_d[h, :, r0:r0 + w], ks[:, 0:w], reads=[kk], writes=["KT_d"])
                    else:
                        p.op("act", lambda e, ks=ks, bank=bank, w=w: e.mul(out=ks[:, 0:w], in_=bank[:, 0:w],
                                                                             mul=QSC), reads=[bkey], writes=[kk])
                        p.ld("sp", QT_d[h, :, r0:r0 + w], ks[:, 0:w], reads=[kk], writes=["QT_d"])
                    cnt3 += 1
            for j in range(nj):
                t = r0 // 128 + j
                v_ = vs[t % 2]
                vk = "vs%d" % (t % 2)
                for hf in range(2):
                    bank = PB[4 + hf]
                    bkey = "pb%d" % (4 + hf)
                    for kc in range(8):
                        p.mm(bank[:], hT3[:, kc, j * 128:(j + 1) * 128], wkv[:, kc, D + hf * 512:D + (hf + 1) * 512],
                             start=(kc == 0), stop=(kc == 7), reads=["hT3", "wkv"], writes=[bkey],
                             signal=(kc == 7))
                    if hf == 0:
                        p.cp("dve", v_[:, 0:512], bank[:], reads=[bkey], writes=[vk])
                    else:
                        p.cp("act", v_[:, 512:1024], bank[:], reads=[bkey], writes=[vk])
                p.ld("sp", V_d[t * 128:(t + 1) * 128, :], v_[:], reads=[vk], writes=["V_d"])
        p.phase_end()

    if stop_phase >= 4:
        p.phase_begin()
        c2 = load_c2(p)
        Ub = p.sb("Ub", [128, 128], BF16)
        Lb = p.sb("Lb", [128, 128], BF16)
        p.cp("dve", Ub[:], c2[:, 2048:2176], reads=["c2"], writes=["Ub"])
        p.cp("dve", Lb[:], cst[:, 128:256], reads=["cst"], writes=["Lb"])
        rowmask = c2[:, 2432:2433]
        mbf = p.sb("mbf", [128, 2048], BF16)
        p.cp("dve", mbf[:], c2[:, 0:2048], reads=["c2"], writes=["mbf"])
        negm = p.sb("negm", [128, 2048], F32)
        p.ts("dve", negm[:], c2[:, 0:2048], 30000.0, -30000.0, ALU.mult, ALU.add, reads=["c2"], writes=["negm"])
        negrow = p.sb("negrow", [128, 2], F32)
        p.ts("dve", negrow[:, 0:1], rowmask, 30000.0, -30000.0, ALU.mult, ALU.add, reads=["c2"], writes=["negrow"])
        p.op("dve", lambda e: e.memset(negrow[:, 1:2], 0.0), reads=["negrow"], writes=["negrow"])
        KTs = [p.sb("KTs%d" % i, [128, RS], BF16) for i in range(4)]
        QTs = [p.sb("QTs%d" % i, [128, RS], BF16) for i in range(4)]
        Vbs = [p.sb("Vbs%d" % i, [128, TPS, 128], BF16) for i in range(4)]
        Eb = [p.sb("Eb%d" % i, [128, 512], F32) for i in range(2)]
        SPB = [p.sb("SPB%d" % i, [128, 512], BF16) for i in range(4)]
        Tb = [p.sb("Tb%d" % i, [128, 512], F32) for i in range(4)]
        AB = [p.sb("AB%d" % i, [128, 512], BF16) for i in range(4)]
        OTs = [p.sb("OTs%d" % i, [128, 512], BF16) for i in range(2)]

        def ld4(h):
            for s_ in range(2):
                sl = (h % 2) * 2 + s_
                rr = slice(s_ * RS, (s_ + 1) * RS)
                p.ld("sp", KTs[sl][:], KT_d[h, :, rr], writes=["KTs%d" % sl])
                p.ld("sp", QTs[sl][:], QT_d[h, :, rr], writes=["QTs%d" % sl])
                p.ld("sp", Vbs[sl][:], V_d[rr, h * 128:(h + 1) * 128].rearrange("(j p) d -> p j d", p=128),
                     writes=["Vbs%d" % sl])

        steps = []
        first_x = {}
        for h in range(8):
            for qi in range(9):
                jmax = min(4 * qi + 3, TPS - 1)
                for j in range(jmax, -1, -1):
                    for s_ in range(2):
                        first_x.setdefault(h, len(steps))
                        steps.append(dict(s=s_, h=h, qi=qi, j=j, jmax=jmax, x=len(steps),
                                          sl=(h % 2) * 2 + s_))
        NS = len(steps)
        Zb = [PB[0], PB[1], PB[6]]
        zkeys = ["pb0", "pb1", "pb6"]
        NDUMMY = 2

        def geo(st):
            q0 = st["qi"] * 512
            W = min(512, RS - q0)
            return q0, W, st["j"] - 4 * st["qi"]

        def stage1(st):
            x, sl, j = st["x"], st["sl"], st["j"]
            q0, W, r = geo(st)
            if x == first_x[st["h"]] + 6 and st["h"] + 1 < 8:
                ld4(st["h"] + 1)
            Z, zk = Zb[x % 3], zkeys[x % 3]
            b2, b3 = x % 2, x % 4
            p.mm(Z[:, 0:W], KTs[sl][:, j * 128:(j + 1) * 128], QTs[sl][:, q0:q0 + W],
                 reads=["KTs%d" % sl, "QTs%d" % sl], writes=[zk])

        def stage1e(st):
            x, sl, j = st["x"], st["sl"], st["j"]
            q0, W, r = geo(st)
            Z, zk = Zb[x % 3], zkeys[x % 3]
            b2, b3 = x % 2, x % 4
            p.act(Eb[b2][:, 0:W], Z[:, 0:W], AF.Exp, reads=[zk], writes=["Eb%d" % b2])
            p.skip_self = True
            p.act(SPB[b3][:, 0:W], Eb[b2][:, 0:W], AF.Ln, bias=1.0, reads=["Eb%d" % b2], writes=["SPB%d" % b3])
            p.skip_self = False

        def stage1b(st):
            x, j = st["x"], st["j"]
            q0, W, r = geo(st)
            Z, zk = Zb[x % 3], zkeys[x % 3]
            b2, b3 = x % 2, x % 4
            if r >= 0:
                p.tt("dve", SPB[b3][:, 0:W], SPB[b3][:, 0:W], mbf[:, r * 512:r * 512 + W], ALU.mult,
                     reads=["SPB%d" % b3, "mbf"], writes=["SPB%d" % b3])
            if j == 0:
                p.ts("dve", SPB[b3][:, 0:W], SPB[b3][:, 0:W], rowmask, None, ALU.mult,
                     reads=["SPB%d" % b3, "c2"], writes=["SPB%d" % b3])
            p.tt("dve", Tb[b3][:, 0:W], Z[:, 0:W], SPB[b3][:, 0:W], ALU.subtract, reads=[zk, "SPB%d" % b3],
                 writes=["Tb%d" % b3])
            if r >= 0:
                p.tt("pool", Tb[b3][:, 0:W], Tb[b3][:, 0:W], negm[:, r * 512:r * 512 + W], ALU.add,
                     reads=["Tb%d" % b3, "negm"], writes=["Tb%d" % b3])

        def stage2a(st):
            x, j = st["x"], st["j"]
            q0, W, r = geo(st)
            b3 = x % 4
            A, ak = PB[2 + st["s"]], "pb%d" % (2 + st["s"])
            p.mm(A[:, 0:W], Ub[:], SPB[b3][:, 0:W], start=(j == st["jmax"]), stop=False,
                 reads=["Ub", "SPB%d" % b3], writes=[ak])
            p.tt("dve", Tb[b3][:, 0:W], Tb[b3][:, 0:W], A[:, 0:W], ALU.subtract, reads=["Tb%d" % b3, ak],
                 writes=["Tb%d" % b3])

        def stage2b(st):
            x, j = st["x"], st["j"]
            q0, W, r = geo(st)
            b3 = x % 4
            A, ak = PB[2 + st["s"]], "pb%d" % (2 + st["s"])
            p.mm(A[:, 0:W], Lb[:], SPB[b3][:, 0:W], start=False, stop=(j == 0),
                 reads=["Lb", "SPB%d" % b3], writes=[ak])

        def stage2c(st):
            x, j = st["x"], st["j"]
            q0, W, r = geo(st)
            b3 = x % 4
            p.act(AB[b3][:, 0:W], Tb[b3][:, 0:W], AF.Exp, bias=(negrow[:, 0:1] if j == 0 else negrow[:, 1:2]),
                  reads=["Tb%d" % b3, "negrow"], writes=["AB%d" % b3])

        def stage3(st):
            x, sl, j = st["x"], st["sl"], st["j"]
            q0, W, r = geo(st)
            b3 = x % 4
            O, ok_ = PB[4 + st["s"]], "pb%d" % (4 + st["s"])
            p.mm(O[:, 0:W], Vbs[sl][:, j, :], AB[b3][:, 0:W], start=(j == st["jmax"]), stop=(j == 0),
                 reads=["Vbs%d" % sl, "AB%d" % b3], writes=[ok_])
            if j == 0:
                ot, otk = OTs[st["s"]], "OTs%d" % st["s"]
                p.cp("act", ot[:, 0:W], O[:, 0:W], reads=[ok_], writes=[otk])
                p.ld("sp", OT_d[st["h"], :, st["s"] * RS + q0:st["s"] * RS + q0 + W], ot[:, 0:W], reads=[otk],
                     writes=["OT_d"])

        ld4(0)
        for n in range(NS + 3):
            if 0 <= n - 2 < NS:
                stage2b(steps[n - 2])
            if 0 <= n - 1 < NS:
                stage2a(steps[n - 1])
            if n == 0:
                stage1(steps[0])
            if n + 1 < NS:
                stage1(steps[n + 1])
            if n < NS:
                stage1e(steps[n])
            for _ in range(NDUMMY):
                p.mm(tpb[:].rearrange("p a b -> p (a b)").bitcast(F32), Ub[:], mbf[:, 0:512], reads=["Ub", "mbf"],
                     writes=["tpb"], signal=False)
            if 0 <= n - 3 < NS:
                stage3(steps[n - 3])
            if 0 <= n - 2 < NS:
                stage2c(steps[n - 2])
            if n < NS:
                stage1b(steps[n])
        p.phase_end()

    rinfo = p.sb("rinfo", [128, NT, 4], F32)
    rpos = p.sb("rpos", [128, NT, 2], I32)
    if stop_phase >= 5:
        p.phase_begin()
        c2 = load_c2(p)
        p.ld("sp", gbc[:], lng[2:3, :].partition_broadcast(128), writes=["gbc"])
        p.ld("sp", bbc[:], lnb[2:3, :].partition_broadcast(128), writes=["bbc"])
        wo = load_w_bf(p, "wo", w_o, D, D, "wo")
        wr = p.sb("wr", [128, 8, NE], F32)
        p.ld("sp", wr[:], w_r.rearrange("(k p) e -> p k e", p=128), writes=["wr"])
        SLb = p.sb("SLb", [128, 128], BF16)
        UIb = p.sb("UIb", [128, 128], BF16)
        p.cp("dve", SLb[:], c2[:, 2176:2304], reads=["c2"], writes=["SLb"])
        p.cp("dve", UIb[:], c2[:, 2304:2432], reads=["c2"], writes=["UIb"])
        rowmask = c2[:, 2432:2433]
        ecap = c2[:, 2440:2448]
        trash = c2[:, 2448:2449]
        OTt = [p.sb("OTt%d" % i, [128, 8, 128], BF16) for i in range(2)]
        xf5 = [p.sb("x5f%d" % i, [128, D], F32) for i in range(2)]
        y5 = p.sb("y5", [128, D], F32)
        hc = [p.sb("hc%d" % i, [128, D], F32) for i in range(2)]
        hcb = [p.sb("hcb%d" % i, [128, D], BF16) for i in range(2)]
        hcT = p.sb("hcT", [128, 8, 128], F32)
        rt = p.sb("rt", [128, 96], F32)
        ohb = p.sb("ohb", [128, 8], BF16)
        RK = pD

        def ld5(t):
            sl = t % 2
            p.ld("sp", OTt[sl][:], OT_d[:, :, t * 128:(t + 1) * 128].rearrange("h p r -> p h r"),
                 reads=["OT_d"], writes=["OTt%d" % sl])
            p.ld("sp", xf5[sl][:], h_b[t * 128:(t + 1) * 128, :], reads=["h_b_d%d" % t], writes=["x5f%d" % sl])

        ld5(0)
        for t in range(NT):
            sl = t % 2
            if t + 1 < NT:
                ld5(t + 1)
            for hf in range(2):
                bank = PB[hf]
                bkey = "pb%d" % hf
                for kc in range(8):
                    p.mm(bank[:], OTt[sl][:, kc, :], wo[:, kc, hf * 512:(hf + 1) * 512], start=(kc == 0),
                         stop=(kc == 7), reads=["OTt%d" % sl, "wo"], writes=[bkey], signal=(kc == 7))
                p.stt(y5[:, hf * 512:(hf + 1) * 512], xf5[sl][:, hf * 512:(hf + 1) * 512], ALPHA, bank[:],
                      ALU.mult, ALU.add, reads=["x5f%d" % sl, bkey], writes=["y5"])
            hk5 = "hc%d" % sl
            layernorm_tile(p, y5[:], hc[sl][:], gbc[:], bbc[:], sm, "y5", hk5, extra_reads=["gbc", "bbc"])
            p.ld("sp", h_c[t * 128:(t + 1) * 128, :], hc[sl][:], reads=[hk5], writes=["h_c_d%d" % t])
            p.cp("pool", hcb[sl][:], hc[sl][:], reads=[hk5], writes=["hcb%d" % sl])
            for kc in range(8):
                bank = PB[4 + kc // 4]
                p.tr(bank[:, (kc % 4) * 128:(kc % 4 + 1) * 128], hc[sl][:, kc * 128:(kc + 1) * 128], ident_f,
                     reads=[hk5, "cst"], writes=["pb%d" % (4 + kc // 4)], signal=(kc % 4 == 3))
            p.cp("act", hcT[:, 0:4, :].rearrange("p k n -> p (k n)"), PB[4][:], reads=["pb4"], writes=["hcT"])
            p.cp("dve", hcT[:, 4:8, :].rearrange("p k n -> p (k n)"), PB[5][:], reads=["pb5"], writes=["hcT"])
            for kc in range(8):
                p.mm(pC[:, 0:NE], hcT[:, kc, :], wr[:, kc, :], start=(kc == 0), stop=(kc == 7),
                     reads=["hcT", "wr"], writes=["pb2"], signal=(kc == 7))
            p.cp("dve", rt[:, 0:8], pC[:, 0:NE], reads=["pb2"], writes=["rt"])
            p.op("dve", lambda e: e.max(out=rt[:, 8:16], in_=rt[:, 0:8]), reads=["rt"], writes=["rt"])
            p.ts("dve", rt[:, 16:24], rt[:, 0:8], rt[:, 8:9], None, ALU.is_equal, reads=["rt"], writes=["rt"])
            p.ts("dve", rt[:, 24:32], rt[:, 0:8], rt[:, 9:10], None, ALU.is_equal, reads=["rt"], writes=["rt"])
            p.tt("dve", rt[:, 32:40], rt[:, 16:24], rt[:, 24:32], ALU.add, reads=["rt"], writes=["rt"])
            if t % TPS == 0:
                p.ts("dve", rt[:, 32:40], rt[:, 32:40], rowmask, None, ALU.mult, reads=["rt", "c2"], writes=["rt"])
            p.cp("dve", ohb[:], rt[:, 32:40], reads=["rt"], writes=["ohb"])
            p.mm(RK[:, 0:NE], SLb[:], ohb[:], start=(t == 0), stop=False, reads=["SLb", "ohb"], writes=["pb3"])
            p.cp("dve", rt[:, 40:48], RK[:, 0:NE], reads=["pb3"], writes=["rt"])
            p.mm(RK[:, 0:NE], UIb[:], ohb[:], start=False, stop=(t == NT - 1), reads=["UIb", "ohb"],
                 writes=["pb3"])
            p.tt("dve", rt[:, 56:57], rt[:, 8:9], rt[:, 9:10], ALU.subtract, reads=["rt"], writes=["rt"])
            p.act(rt[:, 57:58], rt[:, 56:57], AF.Sigmoid, reads=["rt"], writes=["rt"])
            p.ts("dve", rt[:, 58:59], rt[:, 57:58], -1.0, 1.0, ALU.mult, ALU.add, reads=["rt"], writes=["rt"])
            for k2 in range(2):
                oh = rt[:, 16 + 8 * k2:24 + 8 * k2]
                p.tt("dve", rt[:, 48:56], oh, rt[:, 40:48], ALU.mult, reads=["rt"], writes=["rt"])
                p.op("dve", lambda e: e.reduce_sum(out=rt[:, 60:61], in_=rt[:, 48:56], axis=AX.X),
                     reads=["rt"], writes=["rt"])
                p.tt("dve", rt[:, 48:56], oh, ecap, ALU.mult, reads=["rt", "c2"], writes=["rt"])
                p.op("dve", lambda e: e.reduce_sum(out=rt[:, 61:62], in_=rt[:, 48:56], axis=AX.X),
                     reads=["rt"], writes=["rt"])
                p.ts("dve", rt[:, 62:63], rt[:, 60:61], float(CAP), None, ALU.is_lt, reads=["rt"], writes=["rt"])
                if t % TPS == 0:
                    p.tt("dve", rt[:, 62:63], rt[:, 62:63], rowmask, ALU.mult, reads=["rt", "c2"], writes=["rt"])
                p.tt("dve", rt[:, 63:64], rt[:, 60:61], rt[:, 61:62], ALU.add, reads=["rt"], writes=["rt"])
                p.tt("dve", rt[:, 63:64], rt[:, 63:64], trash, ALU.subtract, reads=["rt", "c2"], writes=["rt"])
                p.tt("dve", rt[:, 63:64], rt[:, 63:64], rt[:, 62:63], ALU.mult, reads=["rt"], writes=["rt"])
                p.tt("dve", rt[:, 63:64], rt[:, 63:64], trash, ALU.add, reads=["rt", "c2"], writes=["rt"])
                p.cp("dve", rpos[:, t, k2:k2 + 1], rt[:, 63:64], reads=["rt"], writes=["rpos"])
                p.tt("dve", rinfo[:, t, k2:k2 + 1], rt[:, 57 + k2:58 + k2], rt[:, 62:63], ALU.mult,
                     reads=["rt"], writes=["rinfo"])
                p.dma("pool", lambda e, t=t, k2=k2, sl=sl: e.indirect_dma_start(
                    out=Xslot[:, :], out_offset=bass.IndirectOffsetOnAxis(ap=rpos[:, t, k2:k2 + 1], axis=0),
                    in_=hcb[sl][:], in_offset=None),
                    reads=["hcb%d" % sl, "rpos", "Xslot"], writes=["Xslot_s"])
        p.phase_end()
        dbg_src = h_c
        n_dbg = NT

    if stop_phase >= 6:
        p.phase_begin()
        NTI = CAP // 128
        XT = p.sb("XT", [128, 8, CAP], BF16)
        Y = p.sb("Y", [128, NTI, D], F32)
        xs = [p.sb("xs%d" % i, [128, D], BF16) for i in range(2)]
        wgc = [p.sb("wgc%d" % i, [128, 8, 512], BF16) for i in range(2)]
        wuc = [p.sb("wuc%d" % i, [128, 8, 512], BF16) for i in range(2)]
        wdc = [p.sb("wdc%d" % i, [128, 4, D], BF16) for i in range(2)]
        HT6 = [p.sb("HT6_%d" % i, [128, 4, 512], BF16) for i in range(2)]
        sg6 = [p.sb("sg6_%d" % i, [128, 512], F32) for i in range(2)]
        NCH = DFE // 512
        chunks = [(e_, cf) for e_ in range(NE) for cf in range(NCH)]

        def ldw(ci):
            e_, cf = chunks[ci]
            sl = ci % 2
            cs = slice(cf * 512, (cf + 1) * 512)
            p.ld("pool", wgc[sl][:], w_ge[e_, :, cs].rearrange("(k p) n -> p k n", p=128), writes=["wgc%d" % sl])
            p.ld("pool", wuc[sl][:], w_ue[e_, :, cs].rearrange("(k p) n -> p k n", p=128), writes=["wuc%d" % sl])
            p.ld("pool", wdc[sl][:], w_de[e_, cs, :].rearrange("(f p) n -> p f n", p=128), writes=["wdc%d" % sl])

        ldw(0)
        tg_groups = [(g0, min(512, CAP - g0)) for g0 in range(0, CAP, 512)]
        gcount = 0
        for ci, (e_, cf) in enumerate(chunks):
            sl = ci % 2
            if cf == 0:
                for i in range(NTI):
                    x_ = xs[i % 2]
                    p.ld("sp", x_[:], Xslot[e_ * CAP + i * 128:e_ * CAP + (i + 1) * 128, :],
                         reads=["Xslot", "Xslot_s"], writes=["xs%d" % (i % 2)])
                    transpose_rows(p, x_, XT, i * 128, "xs%d" % (i % 2), "XT")
            if ci + 1 < len(chunks):
                ldw(ci + 1)
            for (g0, gw) in tg_groups:
                hb6 = HT6[gcount % 2]
                hk6 = "HT6_%d" % (gcount % 2)
                for fb in range(4):
                    gb, ub = PB[fb % 2], PB[2 + fb % 2]
                    gk_, uk_ = "pb%d" % (fb % 2), "pb%d" % (2 + fb % 2)
                    for kc in range(8):
                        p.mm(gb[:, 0:gw], wgc[sl][:, kc, fb * 128:(fb + 1) * 128], XT[:, kc, g0:g0 + gw],
                             start=(kc == 0), stop=(kc == 7), reads=["XT", "wgc%d" % sl], writes=[gk_],
                             signal=(kc == 7))
                    for kc in range(8):
                        p.mm(ub[:, 0:gw], wuc[sl][:, kc, fb * 128:(fb + 1) * 128], XT[:, kc, g0:g0 + gw],
                             start=(kc == 0), stop=(kc == 7), reads=["XT", "wuc%d" % sl], writes=[uk_],
                             signal=(kc == 7))
                    p.act(sg6[fb % 2][:, 0:gw], gb[:, 0:gw], AF.Silu, reads=[gk_], writes=["sg6_%d" % (fb % 2)])
                    p.tt("dve", hb6[:, fb, 0:gw], sg6[fb % 2][:, 0:gw], ub[:, 0:gw], ALU.mult,
                         reads=["sg6_%d" % (fb % 2), uk_], writes=[hk6])
                for j in range(gw // 128):
                    ti = g0 // 128 + j
                    for hf in range(2):
                        bank = PB[4 + hf]
                        bkey = "pb%d" % (4 + hf)
                        for fb in range(4):
                            p.mm(bank[:], hb6[:, fb, j * 128:(j + 1) * 128], wdc[sl][:, fb, hf * 512:(hf + 1) * 512],
                                 start=(fb == 0), stop=(fb == 3), reads=[hk6, "wdc%d" % sl], writes=[bkey],
                                 signal=(fb == 3))
                        ysl = Y[:, ti, hf * 512:(hf + 1) * 512]
                        if cf == 0:
                            if hf == 0:
                                p.cp("act", ysl, bank[:], reads=[bkey], writes=["Y"])
                            else:
                                p.cp("pool" if False else "dve", ysl, bank[:], reads=[bkey], writes=["Y"])
                        else:
                            p.tt("dve", ysl, ysl, bank[:], ALU.add, reads=["Y", bkey], writes=["Y"])
                gcount += 1
            if cf == NCH - 1:
                p.ld("sp", Yslot[e_ * CAP:(e_ + 1) * CAP, :].rearrange("(i p) d -> p i d", p=128), Y[:],
                     reads=["Y"], writes=["Yslot"])
        p.phase_end()

    if stop_phase >= 7:
        p.phase_begin()
        p.ld("sp", gbc[:], lng[3:4, :].partition_broadcast(128), writes=["gbc"])
        p.ld("sp", bbc[:], lnb[3:4, :].partition_broadcast(128), writes=["bbc"])
        Y1 = [p.sb("Y1_%d" % i, [128, D], F32) for i in range(2)]
        Y2 = [p.sb("Y2_%d" % i, [128, D], F32) for i in range(2)]
        xf7 = [p.sb("x7f%d" % i, [128, D], F32) for i in range(2)]
        y7 = p.sb("y7", [128, D], F32)
        ob = [p.sb("ob%d" % i, [128, D], F32) for i in range(2)]

        def ld7(t):
            sl = t % 2
            p.ld("sp", xf7[sl][:], h_c[t * 128:(t + 1) * 128, :], reads=["h_c_d%d" % t], writes=["x7f%d" % sl])
            for k2, Yk in enumerate((Y1, Y2)):
                p.dma("pool", lambda e, t=t, k2=k2, Yk=Yk, sl=sl: e.indirect_dma_start(
                    out=Yk[sl][:], out_offset=None, in_=Yslot[:, :],
                    in_offset=bass.IndirectOffsetOnAxis(ap=rpos[:, t, k2:k2 + 1], axis=0)),
                    reads=["Yslot", "Yzero", "rpos"], writes=["Y%d_%d" % (k2 + 1, sl)])

        ld7(0)
        for t in range(NT):
            sl = t % 2
            if t + 1 < NT:
                ld7(t + 1)
            p.ts("dve", y7[:], Y1[sl][:], rinfo[:, t, 0:1], None, ALU.mult, reads=["Y1_%d" % sl, "rinfo"],
                 writes=["y7"])
            p.stt(y7[:], Y2[sl][:], rinfo[:, t, 1:2], y7[:], ALU.mult, ALU.add, reads=["Y2_%d" % sl, "rinfo", "y7"],
                  writes=["y7"])
            p.stt(y7[:], xf7[sl][:], ALPHA, y7[:], ALU.mult, ALU.add, reads=["x7f%d" % sl, "y7"], writes=["y7"])
            layernorm_tile(p, y7[:], ob[sl][:], gbc[:], bbc[:], sm, "y7", "ob%d" % sl, extra_reads=["gbc", "bbc"])
            p.ld("sp", out_d[t * 128:(t + 1) * 128, :], ob[sl][:], reads=["ob%d" % sl], writes=["out%d" % t])
        p.wait_all("sp", ["out%d" % t for t in range(NT)])
        p.phase_end(final=True)
        return nc

    p.phase_begin()
    dbg = p.sb("dbg", [128, D], F32)
    for t in range(n_dbg):
        p.ld("sp", dbg[:], dbg_src[t * 128:(t + 1) * 128, :], writes=["dbg"])
        p.ld("sp", out_d[t * 128:(t + 1) * 128, :], dbg[:], reads=["dbg"], writes=["out%d" % t])
    p.wait_all("sp", ["out%d" % t for t in range(n_dbg)])
    p.phase_end(final=True)
    return nc


def make_consts():
    c = np.zeros((128, 1024), np.float32)
    c[:, 0:128] = np.eye(128, dtype=np.float32)
    s = np.arange(128)[:, None]
    t = np.arange(128)[None, :]
    c[:, 128:256] = (s <= t).astype(np.float32)
    for h in range(4):
        c[h, 256 + h * 128:256 + (h + 1) * 128] = 1.0
    return c


def make_consts2():
    c = np.zeros((128, 4096), np.float32)
    k = np.arange(128)[:, None]
    q = np.arange(512)[None, :]
    for r in range(4):
        c[:, r * 512:(r + 1) * 512] = ((128 * r + k) < q).astype(np.float32)
    kk = np.arange(128)[None, :]
    c[:, 2048:2176] = (k > kk).astype(np.float32)
    c[:, 2176:2304] = (k < kk).astype(np.float32)
    c[:, 2304:2432] = (k >= kk).astype(np.float32)
    c[:, 2432] = (np.arange(128) >= PAD).astype(np.float32)
    c[:, 2440:2448] = (np.arange(NE) * CAP).astype(np.float32)[None, :]
    c[:, 2448] = (NE * CAP + np.arange(128)).astype(np.float32)
    return c


def make_in_maps(inputs):
    x = np.asarray(inputs["x"], np.float32)
    meta = np.asarray(inputs["meta"], np.float32)
    bg = np.asarray(inputs["b_gate_a"], np.float32)[0]
    shared = {
        "w_in": np.ascontiguousarray(inputs["w_in_a"][0]),
        "bgate": np.ascontiguousarray(np.stack([bg[0:4], bg[4:8]], axis=1)),
        "norm_pk": np.ascontiguousarray(np.asarray(inputs["norm_a"], np.float32)[0].reshape(8, 128).T),
        "w_out": np.ascontiguousarray(inputs["w_out_a"][0]),
        "lng": np.ascontiguousarray(np.asarray(inputs["ln_g"], np.float32).reshape(4, D)),
        "lnb": np.ascontiguousarray(np.asarray(inputs["ln_b"], np.float32).reshape(4, D)),
        "consts": make_consts(),
        "consts2": make_consts2(),
        "w_gd": np.ascontiguousarray(inputs["w_gate_d"][0]),
        "w_ud": np.ascontiguousarray(inputs["w_up_d"][0]),
        "w_dd": np.ascontiguousarray(inputs["w_down_d"][0]),
        "w_kv": np.ascontiguousarray(inputs["w_kv"]),
        "w_q": np.ascontiguousarray(inputs["w_q_b"][0]),
        "w_o": np.ascontiguousarray(inputs["w_o_b"][0]),
        "w_r": np.ascontiguousarray(inputs["w_router"][0]),
        "w_ge": np.ascontiguousarray(inputs["w_gate_e"][0]),
        "w_ue": np.ascontiguousarray(inputs["w_up_e"][0]),
        "w_de": np.ascontiguousarray(inputs["w_down_e"][0]),
    }
    maps = []
    for c in range(NCORES):
        hin = np.zeros((2, RS, D), np.float32)
        for s in range(2):
            hin[s, PAD:128] = meta
            hin[s, 128:] = x[2 * c + s]
        m = dict(shared)
        m["hin"] = hin.reshape(R, D)
        maps.append(m)
    return maps


def kernel(**inputs):
    nc = build()
    maps = make_in_maps(inputs)
    res = run_bass_kernel_spmd(nc, maps, core_ids=list(range(NCORES)))
    out = np.zeros((16, SEQ, D), np.float32)
    for c in range(NCORES):
        o = res.results[c]["out"].reshape(2, RS, D)
        out[2 * c:2 * c + 2] = o[:, 128:, :]
    return out
```
